# Optimizing a Trainium2 kernel written in Bass

```python
import jax
import jax.numpy as jnp
from jax import lax
import numpy as np

D_MODEL = 1024
BATCH = 4
SEQ = 8192
DEPTH = 1

CHUNK = 64
NORM_EPS = 1e-6

RWKV_HEAD = 64
RWKV_WIDTH = D_MODEL
RWKV_HEADS = RWKV_WIDTH // RWKV_HEAD
DECAY_LORA = 64
AAA_LORA = 64
GATE_LORA = 128
RWKV_GN_EPS = 64e-5
RWKV_SPLITS = (RWKV_WIDTH, 2 * RWKV_WIDTH, 3 * RWKV_WIDTH, 3 * RWKV_WIDTH + DECAY_LORA, 3 * RWKV_WIDTH + DECAY_LORA + AAA_LORA)
RWKV_COLS = 3 * RWKV_WIDTH + DECAY_LORA + AAA_LORA + GATE_LORA

SSD_WIDTH = 2 * D_MODEL
SSD_HEAD = 64
SSD_HEADS = SSD_WIDTH // SSD_HEAD
SSD_GROUPS = 4
SSD_HEADS_PER_GROUP = SSD_HEADS // SSD_GROUPS
SSD_STATE = 128
SSD_CONV = 4
SSD_CONV_CH = SSD_WIDTH + 2 * SSD_GROUPS * SSD_STATE
SSD_COLS = SSD_WIDTH + SSD_CONV_CH + SSD_HEADS

N_BRANCH = 2
GATE_COLS = N_BRANCH * D_MODEL
IN_COLS = RWKV_COLS + SSD_COLS + GATE_COLS

N_GROUPS = 4
EXP_PER_GROUP = 8
N_EXPERTS = N_GROUPS * EXP_PER_GROUP
TOP_K = 2
D_EXPERT = 512
MOE_BLOCK = 128

kernel_name = 'hybrid_rwkv7_ssd_hmoe_block'


def rmsnorm(x, g):
    xf = x.astype(jnp.float32)
    y = xf * lax.rsqrt(jnp.mean(xf * xf, axis=-1, keepdims=True) + NORM_EPS)
    return (y * g.astype(jnp.float32)).astype(x.dtype)


def token_shift(u):
    return jnp.pad(u[:, :-1], ((0, 0), (1, 0), (0, 0)))


def rwkv7_mix(p, mu, w0, w_decay, a0, w_a, w_g, k_k, k_a, r_k, ln_g, ln_b):
    B_, S_, _ = p.shape
    f32 = jnp.float32
    u = p + mu * (token_shift(p) - p)
    r, k, v, zw, za, zg = jnp.split(u, RWKV_SPLITS, axis=-1)
    w = -jax.nn.softplus(-(w0 + jnp.tanh(zw) @ w_decay)) - 0.5
    decay = jnp.exp(-jnp.exp(w.astype(f32)))
    a = jax.nn.sigmoid(a0 + za @ w_a)
    g = jax.nn.sigmoid(zg) @ w_g

    def heads(t):
        return t.astype(f32).reshape(B_, S_, RWKV_HEADS, RWKV_HEAD)

    kk = heads(k * k_k)
    kk = kk / jnp.maximum(jnp.sqrt(jnp.sum(kk * kk, axis=-1, keepdims=True)), 1e-12)
    k = k * (1.0 + (a - 1.0) * k_a)
    r_h, k_h, v_h, a_h, w_h = heads(r), heads(k), heads(v), heads(a), heads(decay)

    def step(state, inp):
        r_t, w_t, k_t, v_t, kk_t, a_t = inp
        sa = jnp.einsum('bhij,bhj->bhi', state, -kk_t)
        state = (state * w_t[:, :, None, :]
                 + sa[..., None] * (kk_t * a_t)[:, :, None, :]
                 + v_t[..., None] * k_t[:, :, None, :])
        y_t = jnp.einsum('bhij,bhj->bhi', state, r_t)
        return state, y_t

    xs = tuple(jnp.moveaxis(t, 1, 0) for t in (r_h, w_h, k_h, v_h, kk, a_h))
    state0 = jnp.zeros((B_, RWKV_HEADS, RWKV_HEAD, RWKV_HEAD), f32)
    _, ys = lax.scan(step, state0, xs)
    y = jnp.moveaxis(ys, 0, 1)

    mean = jnp.mean(y, axis=-1, keepdims=True)
    var = jnp.mean(jnp.square(y - mean), axis=-1, keepdims=True)
    y = ((y - mean) * lax.rsqrt(var + RWKV_GN_EPS)).reshape(B_, S_, RWKV_WIDTH)
    y = y * ln_g + ln_b
    bonus = jnp.sum(r_h * k_h * r_k, axis=-1, keepdims=True) * v_h
    out = (y + bonus.reshape(B_, S_, RWKV_WIDTH)) * g
    return out.astype(p.dtype)


def causal_dwconv(u, w, b):
    c = u.shape[-1]
    y = lax.conv_general_dilated(u, w[:, None, :], window_strides=(1,), padding=[(SSD_CONV - 1, 0)],
                                 dimension_numbers=('NWC', 'WIO', 'NWC'), feature_group_count=c)
    return y + b


def ssd_chunked(xs, dt, A, Bm, Cm):
    f32 = jnp.float32
    B_, S_, _ = xs.shape
    nc = S_ // CHUNK
    G, Hg, P, N = SSD_GROUPS, SSD_HEADS_PER_GROUP, SSD_HEAD, SSD_STATE
    x = xs.astype(f32).reshape(B_, nc, CHUNK, G, Hg, P)
    dt = dt.reshape(B_, nc, CHUNK, G, Hg)
    Bc = Bm.astype(f32).reshape(B_, nc, CHUNK, G, N)
    Cc = Cm.astype(f32).reshape(B_, nc, CHUNK, G, N)
    a_dt = dt * A.reshape(G, Hg)
    a_cs = jnp.cumsum(a_dt, axis=2)
    xdt = x * dt[..., None]

    seg = a_cs[:, :, :, None] - a_cs[:, :, None, :]
    tril = jnp.tril(jnp.ones((CHUNK, CHUNK), dtype=bool))[None, None, :, :, None, None]
    decay_in = jnp.exp(jnp.where(tril, seg, -jnp.inf))
    cb = jnp.einsum('bclgn,bcsgn->bclsg', Cc, Bc)
    y_diag = jnp.einsum('bclsgh,bcsghp->bclghp', cb[..., None] * decay_in, xdt)

    decay_st = jnp.exp(a_cs[:, :, -1:] - a_cs)
    states = jnp.einsum('bcsgn,bcsghp->bcghpn', Bc, xdt * decay_st[..., None])
    chunk_decay = jnp.exp(a_cs[:, :, -1])

    def step(h, inp):
        s_c, d_c = inp
        return h * d_c[..., None, None] + s_c, h

    h0 = jnp.zeros((B_, G, Hg, P, N), f32)
    _, prev = lax.scan(step, h0, (jnp.moveaxis(states, 1, 0), jnp.moveaxis(chunk_decay, 1, 0)))
    prev = jnp.moveaxis(prev, 0, 1)
    y_off = jnp.einsum('bclgn,bcghpn->bclghp', Cc, prev) * jnp.exp(a_cs)[..., None]
    return (y_diag + y_off).reshape(B_, S_, SSD_WIDTH)


def ssd_mix(p, conv_w, conv_b, dt_bias, a_log, d_skip, norm_g):
    B_, S_, _ = p.shape
    f32 = jnp.float32
    z = p[..., :SSD_WIDTH]
    xbc = p[..., SSD_WIDTH:SSD_WIDTH + SSD_CONV_CH]
    dt_raw = p[..., SSD_WIDTH + SSD_CONV_CH:]
    xbc = jax.nn.silu(causal_dwconv(xbc, conv_w, conv_b))
    xs = xbc[..., :SSD_WIDTH]
    Bm = xbc[..., SSD_WIDTH:SSD_WIDTH + SSD_GROUPS * SSD_STATE]
    Cm = xbc[..., SSD_WIDTH + SSD_GROUPS * SSD_STATE:]
    dt = jax.nn.softplus(dt_raw.astype(f32) + dt_bias.astype(f32))
    A = -jnp.exp(a_log.astype(f32))
    y = ssd_chunked(xs, dt, A, Bm, Cm)
    y = y + (xs.astype(f32).reshape(B_, S_, SSD_HEADS, SSD_HEAD) * d_skip[:, None]).reshape(B_, S_, SSD_WIDTH)
    u = y * jax.nn.silu(z.astype(f32))
    u = u.reshape(B_, S_, SSD_GROUPS, SSD_WIDTH // SSD_GROUPS)
    u = u * lax.rsqrt(jnp.mean(u * u, axis=-1, keepdims=True) + NORM_EPS)
    u = u.reshape(B_, S_, SSD_WIDTH) * norm_g
    return u.astype(p.dtype)


def hier_moe(h, w_rg, b_rg, w_re, b_re, w_gate, w_up, w_down):
    B_, S_, D = h.shape
    T = B_ * S_
    f32 = jnp.float32
    ht = h.reshape(T, D)
    g_logits = (ht @ w_rg + b_rg).astype(f32)
    g_prob = jax.nn.softmax(g_logits, axis=-1)
    grp = jnp.argmax(g_logits, axis=-1)
    g_w = jnp.take_along_axis(g_prob, grp[:, None], axis=1)[:, 0]
    e_logits = (ht @ w_re + b_re).astype(f32).reshape(T, N_GROUPS, EXP_PER_GROUP)
    e_logits = jnp.take_along_axis(e_logits, grp[:, None, None], axis=1)[:, 0]
    e_prob = jax.nn.softmax(e_logits, axis=-1)
    top_p, top_i = lax.top_k(e_prob, TOP_K)
    gate = g_w[:, None] * top_p / jnp.sum(top_p, axis=-1, keepdims=True)
    expert = grp[:, None] * EXP_PER_GROUP + top_i

    n_assign = T * TOP_K
    e_flat = expert.reshape(-1)
    tok_flat = jnp.repeat(jnp.arange(T, dtype=jnp.int32), TOP_K)
    w_flat = gate.reshape(-1)
    order = jnp.argsort(e_flat)
    e_sorted, tok_sorted, w_sorted = e_flat[order], tok_flat[order], w_flat[order]
    counts = jnp.bincount(e_flat, length=N_EXPERTS)
    starts = jnp.cumsum(counts) - counts
    pad_counts = (counts + MOE_BLOCK - 1) // MOE_BLOCK * MOE_BLOCK
    pad_ends = jnp.cumsum(pad_counts)
    pad_starts = pad_ends - pad_counts
    dest = pad_starts[e_sorted] + (jnp.arange(n_assign) - starts[e_sorted])
    n_blocks = n_assign // MOE_BLOCK + N_EXPERTS
    buf_tok = jnp.zeros((n_blocks * MOE_BLOCK,), jnp.int32).at[dest].set(tok_sorted)
    buf_w = jnp.zeros((n_blocks * MOE_BLOCK,), f32).at[dest].set(w_sorted)
    block_exp = jnp.minimum(jnp.searchsorted(pad_ends, jnp.arange(n_blocks) * MOE_BLOCK, side='right'), N_EXPERTS - 1)

    def run_block(args):
        tok, e = args
        xb = ht[tok]
        hid = jax.nn.silu(xb @ w_gate[e]) * (xb @ w_up[e])
        return hid @ w_down[e]

    y_blocks = lax.map(run_block, (buf_tok.reshape(n_blocks, MOE_BLOCK), block_exp))
    y = jnp.zeros((T, D), f32).at[buf_tok].add(y_blocks.reshape(-1, D).astype(f32) * buf_w[:, None])
    return y.reshape(B_, S_, D).astype(h.dtype)


def setup_inputs(seed: int = 0) -> dict:
    key = jax.random.key(seed)
    ks = iter(jax.random.split(key, 40))
    f32 = jnp.float32
    L = DEPTH

    def nrm(shape, scale):
        return jax.random.normal(next(ks), shape, f32) * scale

    def unif(shape, lo, hi):
        return jax.random.uniform(next(ks), shape, f32, lo, hi)

    x = nrm((BATCH, SEQ, D_MODEL), 1.0)
    attn_norm_g = 1.0 + nrm((L, D_MODEL), 0.05)
    w_in = nrm((L, D_MODEL, IN_COLS), D_MODEL ** -0.5)
    b_gate = nrm((L, GATE_COLS), 0.1)
    rwkv_mu = unif((L, RWKV_COLS), 0.2, 0.8)
    rwkv_w0 = unif((L, RWKV_WIDTH), -6.0, -1.0)
    rwkv_w_decay = nrm((L, DECAY_LORA, RWKV_WIDTH), 0.5 * DECAY_LORA ** -0.5)
    rwkv_a0 = nrm((L, RWKV_WIDTH), 0.5)
    rwkv_w_a = nrm((L, AAA_LORA, RWKV_WIDTH), AAA_LORA ** -0.5)
    rwkv_w_g = nrm((L, GATE_LORA, RWKV_WIDTH), GATE_LORA ** -0.5)
    rwkv_k_k = 0.85 + nrm((L, RWKV_WIDTH), 0.05)
    rwkv_k_a = 1.0 + nrm((L, RWKV_WIDTH), 0.05)
    rwkv_r_k = nrm((L, RWKV_HEADS, RWKV_HEAD), 0.1)
    rwkv_ln_g = 1.0 + nrm((L, RWKV_WIDTH), 0.05)
    rwkv_ln_b = nrm((L, RWKV_WIDTH), 0.05)
    w_up_rwkv = nrm((L, RWKV_WIDTH, D_MODEL), RWKV_WIDTH ** -0.5)
    ssd_conv_w = nrm((L, SSD_CONV, SSD_CONV_CH), SSD_CONV ** -0.5)
    ssd_conv_b = nrm((L, SSD_CONV_CH), 0.05)
    dt0 = jnp.exp(unif((L, SSD_HEADS), float(np.log(1e-3)), float(np.log(1e-1))))
    ssd_dt_bias = dt0 + jnp.log(-jnp.expm1(-dt0))
    ssd_a_log = jnp.log(unif((L, SSD_HEADS), 1.0, 16.0))
    ssd_d = 1.0 + nrm((L, SSD_HEADS), 0.1)
    ssd_norm_g = 1.0 + nrm((L, SSD_WIDTH), 0.05)
    w_up_ssd = nrm((L, SSD_WIDTH, D_MODEL), SSD_WIDTH ** -0.5)
    w_out = nrm((L, D_MODEL, D_MODEL), D_MODEL ** -0.5)
    ffn_norm_g = 1.0 + nrm((L, D_MODEL), 0.05)
    w_router_group = nrm((L, D_MODEL, N_GROUPS), D_MODEL ** -0.5)
    b_router_group = nrm((L, N_GROUPS), 0.01)
    w_router_expert = nrm((L, D_MODEL, N_EXPERTS), D_MODEL ** -0.5)
    b_router_expert = nrm((L, N_EXPERTS), 0.01)
    w_exp_gate = nrm((L, N_EXPERTS, D_MODEL, D_EXPERT), D_MODEL ** -0.5)
    w_exp_up = nrm((L, N_EXPERTS, D_MODEL, D_EXPERT), D_MODEL ** -0.5)
    w_exp_down = nrm((L, N_EXPERTS, D_EXPERT, D_MODEL), D_EXPERT ** -0.5)
    final_norm_g = 1.0 + nrm((D_MODEL,), 0.05)
    return {'x': x, 'attn_norm_g': attn_norm_g, 'w_in': w_in, 'b_gate': b_gate,
            'rwkv_mu': rwkv_mu, 'rwkv_w0': rwkv_w0, 'rwkv_w_decay': rwkv_w_decay, 'rwkv_a0': rwkv_a0,
            'rwkv_w_a': rwkv_w_a, 'rwkv_w_g': rwkv_w_g, 'rwkv_k_k': rwkv_k_k, 'rwkv_k_a': rwkv_k_a,
            'rwkv_r_k': rwkv_r_k, 'rwkv_ln_g': rwkv_ln_g, 'rwkv_ln_b': rwkv_ln_b, 'w_up_rwkv': w_up_rwkv,
            'ssd_conv_w': ssd_conv_w, 'ssd_conv_b': ssd_conv_b, 'ssd_dt_bias': ssd_dt_bias,
            'ssd_a_log': ssd_a_log, 'ssd_d': ssd_d, 'ssd_norm_g': ssd_norm_g, 'w_up_ssd': w_up_ssd,
            'w_out': w_out, 'ffn_norm_g': ffn_norm_g, 'w_router_group': w_router_group,
            'b_router_group': b_router_group, 'w_router_expert': w_router_expert,
            'b_router_expert': b_router_expert, 'w_exp_gate': w_exp_gate, 'w_exp_up': w_exp_up,
            'w_exp_down': w_exp_down, 'final_norm_g': final_norm_g}


def reference(x, attn_norm_g, w_in, b_gate, rwkv_mu, rwkv_w0, rwkv_w_decay, rwkv_a0, rwkv_w_a, rwkv_w_g,
              rwkv_k_k, rwkv_k_a, rwkv_r_k, rwkv_ln_g, rwkv_ln_b, w_up_rwkv, ssd_conv_w, ssd_conv_b,
              ssd_dt_bias, ssd_a_log, ssd_d, ssd_norm_g, w_up_ssd, w_out, ffn_norm_g, w_router_group,
              b_router_group, w_router_expert, b_router_expert, w_exp_gate, w_exp_up, w_exp_down,
              final_norm_g):
    for l in range(DEPTH):
        h = rmsnorm(x, attn_norm_g[l])
        proj = h @ w_in[l]
        p_rwkv = proj[..., :RWKV_COLS]
        p_ssd = proj[..., RWKV_COLS:RWKV_COLS + SSD_COLS]
        p_gate = proj[..., RWKV_COLS + SSD_COLS:] + b_gate[l]
        y_a = rwkv7_mix(p_rwkv, rwkv_mu[l], rwkv_w0[l], rwkv_w_decay[l], rwkv_a0[l], rwkv_w_a[l], rwkv_w_g[l],
                        rwkv_k_k[l], rwkv_k_a[l], rwkv_r_k[l], rwkv_ln_g[l], rwkv_ln_b[l])
        y_b = ssd_mix(p_ssd, ssd_conv_w[l], ssd_conv_b[l], ssd_dt_bias[l], ssd_a_log[l], ssd_d[l], ssd_norm_g[l])
        gates = jax.nn.sigmoid(p_gate)
        g_a = gates[..., :D_MODEL]
        g_b = gates[..., D_MODEL:]
        merged = g_a * (y_a @ w_up_rwkv[l]) + g_b * (y_b @ w_up_ssd[l])
        x = x + merged @ w_out[l]
        h2 = rmsnorm(x, ffn_norm_g[l])
        x = x + hier_moe(h2, w_router_group[l], b_router_group[l], w_router_expert[l], b_router_expert[l],
                         w_exp_gate[l], w_exp_up[l], w_exp_down[l])
    return rmsnorm(x, final_norm_g)
```

```python
import numpy as np
from contextlib import ExitStack
import concourse.bass as bass
import concourse.mybir as mybir
from concourse.bass_utils import run_bass_kernel_spmd

F32 = mybir.dt.float32
BF16 = mybir.dt.bfloat16
AF = mybir.ActivationFunctionType
ALU = mybir.AluOpType
AX = mybir.AxisListType

SAME_ENGINE_SYNC = True
EMBED_WAIT = True
DEBUG = False
STOP_AFTER = 99
P2A_NT = 16
SKIP_2A = False
P2A_CHUNK = True
P2A_STAGE = 9

SEQ = 8192
DM = 1024
NT = SEQ // 128
NFM = 66
C0 = -0.6065306597126334


class Buf:
    __slots__ = ("name", "w", "r")

    def __init__(self, name=""):
        self.name = name
        self.w = None
        self.r = {}


class Ring:
    def __init__(self, tiles):
        self.tiles = tiles
        self.bufs = [Buf() for _ in tiles]
        self.i = 0

    def next(self):
        t, b = self.tiles[self.i], self.bufs[self.i]
        self.i = (self.i + 1) % len(self.tiles)
        return t, b


class Sched:
    ENG = ("pe", "act", "dve", "pool", "sp")
    R = 8

    def __init__(self, nc, es):
        self.nc = nc
        self.es = es
        self.prog = {e: [] for e in self.ENG}
        self.sems = []
        self.sem = {e: self._newsem("c_" + e) for e in self.ENG}
        self.cnt = {e: 0 for e in self.ENG}
        self.waited = {e: {} for e in self.ENG}
        self.dq = ("sp", "act", "pool")
        self.dsem = {q: [self._newsem(f"d_{q}{i}") for i in range(self.R)] for q in self.dq}
        self.dn = {q: 0 for q in self.dq}
        self.ninstr = 0
        self._streams = None
        self._cur = None

    def begin_streams(self, n):
        self._streams = [[] for _ in range(n)]

    def stream(self, k):
        self._cur = k

    def end_streams(self, proportional=False):
        streams, self._streams, self._cur = self._streams, None, None
        order = []
        if proportional:
            pos = [0] * len(streams)
            total = sum(len(x) for x in streams)
            for _ in range(total):
                best, bi = None, None
                for i, st in enumerate(streams):
                    if pos[i] < len(st):
                        f = (pos[i] + 0.5) / len(st)
                        if best is None or f < best:
                            best, bi = f, i
                order.append(streams[bi][pos[bi]])
                pos[bi] += 1
        else:
            n = max(len(x) for x in streams)
            for i in range(n):
                for st in streams:
                    if i < len(st):
                        order.append(st[i])
        for kind, args, kw in order:
            if kind == "op":
                self.op(*args, **kw)
            else:
                self.dma(*args, **kw)

    def _newsem(self, name):
        s = self.es.enter_context(self.nc.semaphore(name))
        self.sems.append(s)
        return len(self.sems) - 1

    def _emit_wait(self, e, s, v):
        if self.waited[e].get(s, 0) >= v:
            return
        self.waited[e][s] = v
        sh = self.sems[s]
        self.prog[e].append(lambda eng, sh=sh, v=v: eng.wait_ge(sh, v))
        self.ninstr += 1

    def _waits(self, e, reads, writes, embed=False):
        waits = {}
        for b in reads:
            if b.w is not None:
                s, v = b.w
                waits[s] = max(waits.get(s, 0), v)
        for b in writes:
            if b.w is not None:
                s, v = b.w
                waits[s] = max(waits.get(s, 0), v)
            for s, v in b.r.items():
                waits[s] = max(waits.get(s, 0), v)
        own = self.sem[e]
        pend = []
        for s, v in waits.items():
            if s == own and (e == "pe" or not SAME_ENGINE_SYNC):
                continue
            if self.waited[e].get(s, 0) >= v:
                continue
            pend.append((s, v))
        emb = None
        if embed and EMBED_WAIT and pend:
            emb = pend.pop()
            self.waited[e][emb[0]] = emb[1]
        for s, v in pend:
            self._emit_wait(e, s, v)
        return emb

    def op(self, e, fn, reads=(), writes=(), signal=True):
        assert signal or e == "pe"
        if self._cur is not None and self._streams is not None:
            self._streams[self._cur].append(("op", (e, fn, list(reads), list(writes)), {"signal": signal}))
            return
        emb = self._waits(e, reads, writes, embed=True)
        own = self.sem[e]
        osh = self.sems[own]
        if emb is not None:
            esh, ev = self.sems[emb[0]], emb[1]
            fn0 = fn
            fn = lambda eng, fn0=fn0, esh=esh, ev=ev: fn0(eng)._wait_ge(esh, ev)
        if signal:
            self.cnt[e] += 1
            c = self.cnt[e]
            self.prog[e].append(lambda eng: fn(eng).then_inc(osh, 1))
        else:
            c = self.cnt[e] + 1
            self.prog[e].append(lambda eng: fn(eng))
        self.ninstr += 1
        for b in writes:
            b.w = (own, c)
            b.r = {}
        for b in reads:
            b.r[own] = max(b.r.get(own, 0), c)

    def dma(self, q, out, in_, reads=(), writes=(), **kw):
        if self._cur is not None and self._streams is not None:
            kw2 = dict(kw)
            kw2["reads"] = list(reads)
            kw2["writes"] = list(writes)
            self._streams[self._cur].append(("dma", (q, out, in_), kw2))
            return
        self._waits(q, reads, writes)
        n = self.dn[q]
        self.dn[q] += 1
        s = self.dsem[q][n % self.R]
        val = 16 * (n // self.R + 1)
        sh = self.sems[s]
        if n >= self.R:
            self._emit_wait(q, s, val - 16)
        self.prog[q].append(lambda eng: eng.dma_start(out=out, in_=in_, **kw).then_inc(sh, 16))
        self.ninstr += 1
        for b in writes:
            b.w = (s, val)
            b.r = {}
        for b in reads:
            b.r[s] = max(b.r.get(s, 0), val)

    def barrier(self):
        toks = [(self.sem[e], self.cnt[e]) for e in self.ENG if self.cnt[e] > 0]
        for q in self.dq:
            n = self.dn[q]
            for i in range(self.R):
                uses = (n - i + self.R - 1) // self.R if n > i else 0
                if uses > 0:
                    toks.append((self.dsem[q][i], 16 * uses))
        for e in self.ENG:
            for s, v in toks:
                if s == self.sem[e]:
                    continue
                self._emit_wait(e, s, v)

    def act(self, out, in_, func, reads, writes, **kw):
        self.op("act", lambda e: e.activation(out=out, in_=in_, func=func, **kw), reads, writes)

    def tt(self, eng, out, in0, in1, op, reads, writes):
        self.op(eng, lambda e: e.tensor_tensor(out=out, in0=in0, in1=in1, op=op), reads, writes)

    def ts(self, eng, out, in0, s1, s2, op0, op1, reads, writes):
        if s2 is None:
            self.op(eng, lambda e: e.tensor_scalar(out=out, in0=in0, scalar1=s1, scalar2=None, op0=op0), reads, writes)
        else:
            self.op(eng, lambda e: e.tensor_scalar(out=out, in0=in0, scalar1=s1, scalar2=s2, op0=op0, op1=op1), reads, writes)

    def stt(self, eng, out, in0, scalar, in1, op0, op1, reads, writes):
        self.op(eng, lambda e: e.scalar_tensor_tensor(out=out, in0=in0, scalar=scalar, in1=in1, op0=op0, op1=op1), reads, writes)

    def copy(self, eng, out, in_, reads, writes):
        if eng == "act":
            self.op(eng, lambda e: e.activation(out=out, in_=in_, func=AF.Copy), reads, writes)
        else:
            self.op(eng, lambda e: e.tensor_copy(out=out, in_=in_), reads, writes)

    def memset(self, eng, ap, val, writes):
        self.op(eng, lambda e: e.memset(ap, val), (), writes)

    def mm(self, out, lhsT, rhs, start, stop, reads, writes, signal=True):
        self.op("pe", lambda e: e.matmul(out, lhsT=lhsT, rhs=rhs, start=start, stop=stop), reads, writes, signal=signal)

    def tr(self, out, in_, ident, reads, writes, signal=True):
        self.op("pe", lambda e: e.transpose(out=out, in_=in_, identity=ident), reads, writes, signal=signal)

    def reduce(self, eng, out, in_, op, reads, writes):
        self.op(eng, lambda e: e.tensor_reduce(out=out, in_=in_, axis=AX.X, op=op), reads, writes)

    def scan(self, out, d0, d1, init, op0, op1, reads, writes):
        self.op("dve", lambda e: e.tensor_tensor_scan(out=out, data0=d0, data1=d1, initial=init, op0=op0, op1=op1), reads, writes)

    def asel(self, out, in_, pattern, cmp, fill, base, cm, reads, writes):
        self.op("pool", lambda e: e.affine_select(out=out, in_=in_, pattern=pattern, compare_op=cmp, fill=fill, base=base, channel_multiplier=cm), reads, writes)

    def build(self):
        nc = self.nc
        with nc.Block() as block:
            @block.tensor
            def _(e):
                for f in self.prog["pe"]:
                    f(e)

            @block.scalar
            def _(e):
                for f in self.prog["act"]:
                    f(e)

            @block.vector
            def _(e):
                for f in self.prog["dve"]:
                    f(e)

            @block.gpsimd
            def _(e):
                for f in self.prog["pool"]:
                    f(e)

            @block.sync
            def _(e):
                for f in self.prog["sp"]:
                    f(e)


class Ctx:
    pass


_UID = [0]


def _alloc(nc, es):
    _UID[0] += 1
    u = _UID[0]

    def sb(name, shape, dt):
        return es.enter_context(nc.sbuf_tensor(f"{name}_u{u}", shape, dt))

    def ps(name, shape, dt):
        return es.enter_context(nc.psum_tensor(f"{name}_u{u}", shape, dt))

    return sb, ps


def phase1(cx):
    nc, S, I, D = cx.nc, cx.S, cx.inp, cx.scr
    with ExitStack() as es:
        sb, ps = _alloc(nc, es)
        hT = sb("hT", [128, 8, SEQ], BF16)
        b_hT = [Buf() for _ in range(NT)]
        gT = sb("p1_gT", [128, 8], F32); b_g = Buf()
        pv = sb("p1_pv", [128, NFM, 5], F32); b_pv = Buf()
        dtb = sb("p1_dtb", [128, 32], F32); b_dtb = Buf()
        S.dma("sp", gT[:], I["attn_gT"], writes=[b_g])
        S.dma("sp", pv[:], I["pv_fm"], writes=[b_pv])
        S.dma("sp", dtb[:], I["dt_bias_b"], writes=[b_dtb])
        xr = Ring([sb(f"p1_x{i}", [128, DM], F32) for i in range(3)])
        xsr = Ring([sb(f"p1_xs{i}", [128, DM], F32) for i in range(2)])
        ssr = Ring([sb(f"p1_ss{i}", [128, 2], F32) for i in range(2)])
        banks = Ring([ps(f"p1_bank{i}", [128, 512], F32) for i in range(8)])
        for i in range(NT):
            if i % 2 == 0:
                S.begin_streams(2)
            S.stream(i % 2)
            x_t, bx = xr.next()
            S.dma("sp", x_t[:], I["xb"][i * 128:(i + 1) * 128, :], writes=[bx])
            xs_t, bxs = xsr.next()
            ss_t, bss = ssr.next()
            S.memset("pool", ss_t[:], 0.0, [bss])
            S.act(xs_t[:], x_t[:], AF.Square, [bx], [bxs, bss], accum_out=ss_t[:, 0:1])
            S.ts("dve", ss_t[:, 1:2], ss_t[:, 0:1], 1.0 / DM, 1e-6, ALU.mult, ALU.add, [bss], [bss])
            S.act(ss_t[:, 1:2], ss_t[:, 1:2], AF.Sqrt, [bss], [bss])
            S.op("dve", lambda e, o=ss_t[:, 1:2]: e.reciprocal(out=o, in_=o), [bss], [bss])
            S.act(xs_t[:], x_t[:], AF.Copy, [bx, bss], [bxs], scale=ss_t[:, 1:2])
            p0, bp0 = banks.next()
            p1, bp1 = banks.next()
            for k in range(8):
                pp, bp = (p0, bp0) if k < 4 else (p1, bp1)
                S.tr(pp[:, (k % 4) * 128:(k % 4 + 1) * 128], xs_t[:, k * 128:(k + 1) * 128], cx.identf[:], [bxs, cx.b_const], [bp])
            for hf, (pp, bp) in enumerate(((p0, bp0), (p1, bp1))):
                S.tt("dve", hT[:, hf * 4:hf * 4 + 4, i * 128:(i + 1) * 128],
                     pp[:].rearrange("p (k t) -> p k t", t=128),
                     gT[:, hf * 4:hf * 4 + 4].unsqueeze(2).to_broadcast([128, 4, 128]),
                     ALU.mult, [bp, b_g], [b_hT[i]])
            if i % 2 == 1:
                S.end_streams()
        wr = Ring([sb(f"p1_w{i}", [128, 8, 128], BF16) for i in range(2)])
        wfm = I["wfm"].rearrange("(k p) c -> p k c", p=128)
        st_l = Ring([sb(f"p1_stl{i}", [128, 516], F32) for i in range(5)])
        tmp = Ring([sb(f"p1_tmp{i}", [128, 512], F32) for i in range(4)])
        o32 = Ring([sb(f"p1_o32{i}", [128, 512], F32) for i in range(4)])
        o16 = Ring([sb(f"p1_o16{i}", [128, 512], BF16) for i in range(4)])
        print("phase1 sbuf remaining", nc.sbuf_bytes_remaining)
        for blk in range(NFM):
            w_t, bw = wr.next()
            S.dma("pool", w_t[:], wfm[:, :, blk * 128:(blk + 1) * 128], writes=[bw])
            kind = "lerp" if blk < 26 else ("conv" if blk < 50 else "gate")
            ncar = {"lerp": 1, "conv": 3, "gate": 0}[kind]
            for tt_ in range(16):
                if tt_ % 4 == 0:
                    S.begin_streams(4)
                S.stream(tt_ % 4)
                pp, bp = banks.next()
                tok = slice(tt_ * 512, (tt_ + 1) * 512)
                for k in range(8):
                    S.mm(pp[:], w_t[:, k, :], hT[:, k, tok], k == 0, k == 7,
                         [bw] + b_hT[4 * tt_:4 * tt_ + 4], [bp], signal=(k == 7))
                if kind == "gate":
                    o, bo = o16.next()
                    S.act(o[:], pp[:], AF.Sigmoid, [bp, b_pv], [bo], bias=pv[:, blk, 4:5])
                    S.dma("sp", D["GA"][blk - 50, :, tok], o[:], [bo], [cx.b_GA[blk - 50][tt_]])
                    if tt_ % 4 == 3:
                        S.end_streams()
                    continue
                st, bst = st_l.next()
                if tt_ == 0:
                    S.memset("pool", st[:, 0:ncar], 0.0, [bst])
                S.copy("act", st[:, ncar:ncar + 512], pp[:], [bp], [bst])
                nst, bnst = st_l.tiles[st_l.i], st_l.bufs[st_l.i]
                if tt_ < 15:
                    S.copy("pool", nst[:, 0:ncar], st[:, 512:512 + ncar], [bst], [bnst])
                if kind == "lerp":
                    d, bd = tmp.next()
                    S.tt("dve", d[:], st[:, 0:512], st[:, 1:513], ALU.subtract, [bst], [bd])
                    o, bo = o32.next()
                    S.stt("dve", o[:], d[:], pv[:, blk, 0:1], st[:, 1:513], ALU.mult, ALU.add, [bd, bst, b_pv], [bo])
                    S.dma("sp", D["RW"][blk, :, tok], o[:], [bo], [cx.b_RW[blk][tt_]])
                else:
                    a, ba = tmp.next()
                    S.ts("dve", a[:], st[:, 0:512], pv[:, blk, 0:1], pv[:, blk, 4:5], ALU.mult, ALU.add, [bst, b_pv], [ba])
                    for k in range(1, 4):
                        S.stt("dve", a[:], st[:, k:k + 512], pv[:, blk, k:k + 1], a[:], ALU.mult, ALU.add, [bst, ba, b_pv], [ba])
                    o, bo = o16.next()
                    S.act(o[:], a[:], AF.Silu, [ba], [bo])
                    S.dma("sp", D["XBC"][blk - 26, :, tok], o[:], [bo], [cx.b_XBC[blk - 26][tt_]])
                if tt_ % 4 == 3:
                    S.end_streams()
        wz = sb("p1_wz", [128, 8, 1024], BF16); b_wz = Buf()
        wdt = sb("p1_wdt", [128, 8, 32], BF16); b_wdt = Buf()
        S.dma("pool", wdt[:], I["wdt"].rearrange("(k p) c -> p k c", p=128), writes=[b_wdt])
        dtr = Ring([sb(f"p1_dt{i}", [128, 32], F32) for i in range(2)])
        for i in range(NT):
            tok = slice(i * 128, (i + 1) * 128)
            pp, bp = banks.next()
            for k in range(8):
                S.mm(pp[:, 0:32], hT[:, k, tok], wdt[:, k, :], k == 0, k == 7, [b_wdt, b_hT[i]], [bp], signal=(k == 7))
            d_t, bd = dtr.next()
            S.tt("dve", d_t[:], pp[:, 0:32], dtb[:], ALU.add, [bp, b_dtb], [bd])
            S.act(d_t[:], d_t[:], AF.Exp, [bd], [bd])
            S.act(d_t[:], d_t[:], AF.Ln, [bd], [bd], bias=1.0)
            S.dma("sp", D["DT"][tok, :], d_t[:], [bd], [cx.b_DT[i]])
        for zh in range(2):
            S.dma("pool", wz[:], I["wz"][:, zh * 1024:(zh + 1) * 1024].rearrange("(k p) c -> p k c", p=128), writes=[b_wz])
            for i in range(NT):
                tok = slice(i * 128, (i + 1) * 128)
                for hf in range(2):
                    pp, bp = banks.next()
                    for k in range(8):
                        S.mm(pp[:], hT[:, k, tok], wz[:, k, hf * 512:(hf + 1) * 512], k == 0, k == 7,
                             [b_wz, b_hT[i]], [bp], signal=(k == 7))
                    o, bo = o16.next()
                    S.act(o[:], pp[:], AF.Silu, [bp], [bo])
                    cz = slice(zh * 1024 + hf * 512, zh * 1024 + (hf + 1) * 512)
                    S.dma("sp", D["SZ"][tok, cz], o[:], [bo], [cx.b_SZ[i]])
    S.barrier()


class NS:
    pass


def phase2a(cx, half):
    nc, S, I, D = cx.nc, cx.S, cx.inp, cx.scr
    with ExitStack() as es:
        sb, ps = _alloc(nc, es)
        _bk = [ps(f"p2_bank{i}", [128, 512], F32) for i in range(8)]
        banks_g = [Ring(_bk[0:2]), Ring(_bk[2:4])]
        banks_t = Ring(_bk[4:6])
        banks_p = Ring(_bk[6:8])
        banks = banks_p
        bc = Buf()
        identb = sb("a_identb", [128, 128], BF16)
        S.copy("pool", identb[:], cx.identf[:], [cx.b_const], [bc])
        ones_f = sb("a_ones", [128, 128], F32)
        S.memset("pool", ones_f[:], 1.0, [bc])
        mask2 = sb("a_mask2", [128, 2, 2, 128], F32)
        mLs = sb("a_mLs", [128, 128], F32)
        mLs4 = sb("a_mLs4", [128, 4, 128], F32)
        ident4 = sb("a_ident4", [128, 4, 128], BF16)
        S.memset("pool", mask2[:], 1.0, [bc])
        S.memset("pool", mLs[:], 1.0, [bc])
        for x in range(2):
            S.asel(mask2[:, x, 0, :], mask2[:, x, 0, :], [[1, 128]], ALU.is_gt, 0.0, 0, -1, [bc], [bc])
            S.asel(mask2[:, x, 1, :], mask2[:, x, 1, :], [[1, 128]], ALU.is_ge, 0.0, 0, -1, [bc], [bc])
        S.asel(mLs[:], mLs[:], [[-1, 128]], ALU.is_gt, 0.0, 0, 1, [bc], [bc])
        for x in range(4):
            S.copy("pool", mLs4[:, x, :], mLs[:], [bc], [bc])
            S.copy("pool", ident4[:, x, :], identb[:], [bc], [bc])
        blockones = sb("a_bones", [128, 128], BF16)
        blocksel = sb("a_bsel", [128, 2], BF16)
        S.memset("pool", blockones[:], 0.0, [bc])
        S.memset("pool", blocksel[:], 0.0, [bc])
        S.memset("pool", blockones[0:64, 0:64], 1.0, [bc])
        S.memset("pool", blockones[64:128, 64:128], 1.0, [bc])
        S.memset("pool", blocksel[0:64, 0:1], 1.0, [bc])
        S.memset("pool", blocksel[64:128, 1:2], 1.0, [bc])
        pvr = sb("a_pvr", [128, 4, 5], F32)
        wl_w = sb("a_wl_w", [128, 512], BF16)
        wl_a = sb("a_wl_a", [128, 512], BF16)
        wg = sb("a_wg", [128, 512], BF16)
        lng = sb("a_lng", [128, 512], F32)
        lnb = sb("a_lnb", [128, 512], F32)
        hc = slice(512 * half, 512 * half + 512)
        S.dma("sp", pvr[:], I["pv_rw"][:, 4 * half:4 * half + 4, :], writes=[bc])
        npvr = sb("a_npvr", [128, 4, 2], F32)
        S.ts("dve", npvr[:], pvr[:, :, 0:2], -1.0, None, ALU.mult, None, [bc], [bc])
        S.dma("pool", wl_w[:], I["wlora_w"][:, hc], writes=[bc])
        S.dma("pool", wl_a[:], I["wlora_a"][:, hc], writes=[bc])
        S.dma("pool", wg[:], I["wg"][:, hc], writes=[bc])
        S.dma("sp", lng[:], I["lng_b"][:, hc], writes=[bc])
        S.dma("sp", lnb[:], I["lnb_b"][:, hc], writes=[bc])
        Sm = sb("a_Sm", [128, 4, 64], F32)
        Sb = sb("a_Sb", [128, 4, 2, 64], BF16)
        bSm = [Buf() for _ in range(4)]
        bSb = [Buf() for _ in range(2)]
        S.memset("pool", Sm[:], 0.0, bSm)
        S.memset("pool", Sb[:], 0.0, bSb)

        ws = []
        for i in range(1):
            W = NS()
            for nm in ("r", "k", "v", "sg", "a", "kkr", "rn", "kp", "beta", "cs", "ce", "P", "Pinv"):
                setattr(W, nm, sb(f"a_w{i}_{nm}", [128, 512], F32))
                setattr(W, "b_" + nm, Buf())
            W.kkn, W.b_kkn = W.kkr, W.b_kkr
            W.t1, W.b_t1 = W.ce, W.b_ce
            W.csm, W.b_csm = W.rn, W.b_rn
            W.Eend, W.b_Eend = W.a, W.b_a
            W.Pprev, W.b_Pprev = W.k, W.b_k
            W.sq = sb(f"a_w{i}_sq", [128, 512], BF16)
            W.b_sq = Buf()
            ws.append(W)
        outs = []
        for par in range(2):
            row = []
            for hp in range(4):
                O = NS()
                O.buf = Buf()
                O.aq = sb(f"a_o{par}{hp}_aq", [128, 4, 2, 128], BF16)
                for nm in ("Kh", "Bh", "vb", "rkr"):
                    setattr(O, nm, sb(f"a_o{par}{hp}_{nm}", [128, 512], BF16))
                O.btz = [sb(f"a_o{par}{hp}_btz{z}", [128, 512], BF16) for z in range(2)]
                O.ktz = [sb(f"a_o{par}{hp}_ktz{z}", [128, 512], BF16) for z in range(2)]
                for z in range(2):
                    S.memset("pool", O.btz[z][:], 0.0, [O.buf])
                    S.memset("pool", O.ktz[z][:], 0.0, [O.buf])
                O.PC = sb(f"a_o{par}{hp}_PC", [128, 4], F32)
                row.append(O)
            outs.append(row)
        shared = []
        for par in range(2):
            H = NS()
            if par == 0:
                H.zz = sb(f"a_s{par}_zz", [128, 512], F32)
                H.zg = sb(f"a_s{par}_zg", [128, 512], F32)
                H.b_zz, H.b_zg = Buf(), Buf()
            else:
                H.zz, H.zg, H.b_zz, H.b_zg = shared[0].zz, shared[0].zg, shared[0].b_zz, shared[0].b_zg
            H.tzw = sb(f"a_s{par}_tzw", [128, 512], BF16)
            H.sgz = sb(f"a_s{par}_sgz", [128, 512], BF16)
            H.buf = Buf()
            shared.append(H)

        def pre_shared(T):
            tok = slice(T * 512, (T + 1) * 512)
            H = shared[T % 2]
            S.dma("sp", H.zz[:], D["RW"][24, :, tok], [cx.b_RW[24][T]], [H.b_zz])
            S.dma("sp", H.zg[:], D["RW"][25, :, tok], [cx.b_RW[25][T]], [H.b_zg])
            S.copy("pool", H.tzw[64:128, :], H.zz[64:128, :], [H.b_zz], [H.buf])
            zt_ = H.zz[0:64, :]
            S.act(zt_, zt_, AF.Exp, [H.b_zz], [H.b_zz], scale=-2.0)
            S.ts("dve", zt_, zt_, 1.0, None, ALU.add, None, [H.b_zz], [H.b_zz])
            S.op("dve", lambda e, o=zt_: e.reciprocal(out=o, in_=o), [H.b_zz], [H.b_zz])
            S.ts("dve", zt_, zt_, 2.0, -1.0, ALU.mult, ALU.add, [H.b_zz], [H.b_zz])
            S.copy("pool", H.tzw[0:64, :], zt_, [H.b_zz], [H.buf])
            S.act(H.zg[:], H.zg[:], AF.Exp, [H.b_zg], [H.b_zg], scale=-1.0)
            S.ts("dve", H.zg[:], H.zg[:], 1.0, None, ALU.add, None, [H.b_zg], [H.b_zg])
            S.op("dve", lambda e, o=H.zg[:]: e.reciprocal(out=o, in_=o), [H.b_zg], [H.b_zg])
            S.copy("pool", H.sgz[:], H.zg[:], [H.b_zg], [H.buf])

        def v3(t):
            return t[:].rearrange("p (c t) -> p c t", t=128)

        def pre(T, hp):
            tok = slice(T * 512, (T + 1) * 512)
            O = outs[T % 2][hp]
            W = ws[hp % len(ws)]
            H = shared[T % 2]
            ghp = 4 * half + hp
            S.dma("sp", W.r[:], D["RW"][ghp, :, tok], [cx.b_RW[ghp][T]], [W.b_r])
            S.dma("sp", W.k[:], D["RW"][8 + ghp, :, tok], [cx.b_RW[8 + ghp][T]], [W.b_k])
            S.dma("sp", W.v[:], D["RW"][16 + ghp, :, tok], [cx.b_RW[16 + ghp][T]], [W.b_v])
            cs_ = slice(hp * 128, (hp + 1) * 128)
            pw, bpw = banks.next()
            S.mm(pw[:], wl_w[:, cs_], H.tzw[:], True, True, [bc, H.buf], [bpw])
            S.act(W.sg[:], pw[:], AF.Exp, [bpw, bc], [W.b_sg], bias=npvr[:, hp, 0:1], scale=-1.0)
            S.ts("dve", W.sg[:], W.sg[:], 1.0, None, ALU.add, None, [W.b_sg], [W.b_sg])
            S.op("dve", lambda e, o=W.sg[:]: e.reciprocal(out=o, in_=o), [W.b_sg], [W.b_sg])
            pa, bpa = banks.next()
            S.mm(pa[:], wl_a[:, cs_], H.tzw[:], True, True, [bc, H.buf], [bpa])
            S.act(W.a[:], pa[:], AF.Exp, [bpa, bc], [W.b_a], bias=npvr[:, hp, 1:2], scale=-1.0)
            S.ts("dve", W.a[:], W.a[:], 1.0, None, ALU.add, None, [W.b_a], [W.b_a])
            S.op("dve", lambda e, o=W.a[:]: e.reciprocal(out=o, in_=o), [W.b_a], [W.b_a])
            S.ts("dve", W.kkr[:], W.k[:], pvr[:, hp, 2:3], None, ALU.mult, None, [W.b_k, bc], [W.b_kkr])
            S.act(W.sq[:], W.kkr[:], AF.Square, [W.b_kkr], [W.b_sq])
            pss, bpss = banks.next()
            S.mm(pss[:], blockones[:], W.sq[:], True, True, [bc, W.b_sq], [bpss])
            S.ts("dve", W.rn[:], pss[:], 1e-24, None, ALU.max, None, [bpss], [W.b_rn])
            S.act(W.rn[:], W.rn[:], AF.Ln, [W.b_rn], [W.b_rn])
            S.act(W.rn[:], W.rn[:], AF.Exp, [W.b_rn], [W.b_rn], scale=-0.5)
            S.tt("dve", W.kkn[:], W.kkr[:], W.rn[:], ALU.mult, [W.b_kkr, W.b_rn, W.b_sq], [W.b_kkn])
            S.ts("dve", W.t1[:], W.a[:], -1.0, pvr[:, hp, 3:4], ALU.add, ALU.mult, [W.b_a, bc], [W.b_t1])
            S.stt("dve", W.kp[:], W.t1[:], 1.0, W.k[:], ALU.add, ALU.mult, [W.b_t1, W.b_k], [W.b_kp])
            S.tt("pool", W.beta[:], W.kkn[:], W.a[:], ALU.mult, [W.b_kkn, W.b_a], [W.b_beta])
            for c in range(4):
                cc = slice(c * 128, (c + 1) * 128)
                S.scan(W.cs[:, cc], ones_f[:, 0:128], W.sg[:, cc], 0.0, ALU.mult, ALU.add, [bc, W.b_sg], [W.b_cs])
            S.act(W.P[:], W.cs[:], AF.Exp, [W.b_cs], [W.b_P], scale=C0)
            S.act(W.Pinv[:], W.cs[:], AF.Exp, [W.b_cs], [W.b_Pinv], scale=-C0)
            S.tt("pool", W.csm[:], W.cs[:], W.sg[:], ALU.subtract, [W.b_cs, W.b_sg], [W.b_csm])
            S.act(W.Pprev[:], W.csm[:], AF.Exp, [W.b_csm], [W.b_Pprev], scale=C0)
            cs3 = v3(W.cs)
            S.tt("dve", v3(W.ce), cs3[:, :, 127:128].to_broadcast([128, 4, 128]), cs3, ALU.subtract, [W.b_cs], [W.b_ce])
            S.act(W.Eend[:], W.ce[:], AF.Exp, [W.b_ce], [W.b_Eend], scale=C0)
            S.act(O.PC[:], cs3[:, :, 127], AF.Exp, [W.b_cs], [O.buf], scale=C0)
            S.stt("dve", O.aq[:, :, 0, :], v3(W.kkn), -1.0, v3(W.Pprev), ALU.mult, ALU.mult, [W.b_kkn, W.b_Pprev], [O.buf])
            S.tt("pool", O.aq[:, :, 1, :], v3(W.r), v3(W.P), ALU.mult, [W.b_r, W.b_P], [O.buf])
            for z in range(2):
                zr = slice(64 * z, 64 * z + 64)
                S.tt("pool", O.ktz[z][zr, :], W.kp[zr, :], W.Pinv[zr, :], ALU.mult, [W.b_kp, W.b_Pinv], [O.buf])
                S.tt("pool", O.btz[z][zr, :], W.beta[zr, :], W.Pinv[zr, :], ALU.mult, [W.b_beta, W.b_Pinv], [O.buf])
            S.tt("pool", O.Kh[:], W.kp[:], W.Eend[:], ALU.mult, [W.b_kp, W.b_Eend], [O.buf])
            S.tt("pool", O.Bh[:], W.beta[:], W.Eend[:], ALU.mult, [W.b_beta, W.b_Eend], [O.buf])
            S.copy("pool", O.vb[:], W.v[:], [W.b_v], [O.buf])
            S.stt("dve", O.rkr[:], W.r[:], pvr[:, hp, 4:5], W.kp[:], ALU.mult, ALU.mult, [W.b_r, W.b_kp, bc], [O.buf])

        tokr = Ring([sb(f"a_tok{i}", [128, 3, 128], BF16) for i in range(8)])
        Dtr = Ring([sb(f"a_Dt{i}", [128, 4, 2], F32) for i in range(2)])
        ATr_g = [Ring([sb(f"a_AT{g}{i}", [128, 4, 2, 2, 128], BF16) for i in range(2)]) for g in range(2)]
        Mr_g = [Ring([sb(f"a_M{g}{i}", [128, 4, 128], BF16) for i in range(3)]) for g in range(2)]
        Nr_g = [Ring([sb(f"a_N{g}{i}", [128, 4, 128], BF16) for i in range(3)]) for g in range(2)]
        Tr_g = [Ring([sb(f"a_T{g}{i}", [128, 4, 128], BF16) for i in range(3)]) for g in range(2)]
        Wbr_g = [Ring([sb(f"a_Wb{g}{i}", [128, 4, 64], BF16) for i in range(1)]) for g in range(2)]
        Ubr_g = [Ring([sb(f"a_Ub{g}{i}", [128, 4, 64], BF16) for i in range(1)]) for g in range(2)]
        ybr = Ring([sb(f"a_yb{i}", [128, 8, 64], F32) for i in range(2)])
        bonr = Ring([sb(f"a_bon{i}", [128, 8, 64], F32) for i in range(2)])
        ycr = Ring([sb(f"a_yc{i}", [128, 8, 64], F32) for i in range(1)])
        sqr = Ring([sb(f"a_sq{i}", [128, 8, 64], F32) for i in range(1)])
        st8 = Ring([sb(f"a_st8{i}", [128, 2, 8], F32) for i in range(2)])
        yar = Ring([sb(f"a_ya{i}", [128, 512], BF16) for i in range(2)])
        yaTr = Ring([sb(f"a_yaT{i}", [128, 4, 128], BF16) for i in range(2)])
        maskflat = mask2[:].rearrange("p a b t -> p (a b t)")

        def head(T, c):
            banks = banks_p
            tc = slice(c * 128, (c + 1) * 128)
            par = T % 2
            toks = []
            Dt, bDt = Dtr.next()
            for hp in range(4):
                O = outs[par][hp]
                pb_, bpb = banks.next()
                pbv = pb_[:].bitcast(BF16)
                for j, src in enumerate((O.Kh, O.Bh, O.vb)):
                    S.tr(pbv[:, j * 128:(j + 1) * 128], src[:, tc], identb[:], [O.buf, bc], [bpb], signal=(j == 2))
                tk, btk = tokr.next()
                S.copy("act", tk[:].rearrange("p a t -> p (a t)"), pbv[:, 0:384], [bpb], [btk])
                toks.append((tk, btk))
                pd, bpd = banks.next()
                S.mm(pd[:, 0:2], O.rkr[:, tc], blocksel[:], True, True, [O.buf, bc], [bpd])
                S.copy("dve", Dt[:, hp, :], pd[:, 0:2], [bpd], [bDt])
            return toks, Dt, bDt

        def chunk(T, c, extra, hd):
            toks, Dt, bDt = hd
            tc = slice(c * 128, (c + 1) * 128)
            par = T % 2
            ci = T * 4 + c
            gtok = slice(ci * 128, (ci + 1) * 128)
            yb, byb = ybr.next()
            bon, bbon = bonr.next()
            if P2A_STAGE < 1.2:
                return
            S.begin_streams(2 + len(extra))
            for g in range(2):
                S.stream(g)
                banks = banks_g[g]
                ATr, Mr, Nr, Tr, Wbr, Ubr = ATr_g[g], Mr_g[g], Nr_g[g], Tr_g[g], Wbr_g[g], Ubr_g[g]
                heads = []
                for hl in range(4):
                    hp = 2 * g + hl // 2
                    h2 = hl % 2
                    heads.append((hl, hp, h2, slice(64 * h2, 64 * h2 + 64), outs[par][hp]))
                AT, bAT = ATr.next()
                for hl, hp, h2, pr, O in heads:
                    pA, bpA = banks.next()
                    aqh = O.aq[:, c, :, :].rearrange("p a t -> p (a t)")
                    S.mm(pA[:, 0:256], O.btz[h2][:, tc], aqh, True, True, [O.buf], [bpA], signal=False)
                    S.mm(pA[:, 256:512], O.ktz[h2][:, tc], aqh, True, True, [O.buf], [bpA])
                    S.tt("dve", AT[:, hl].rearrange("p a b t -> p (a b t)"), pA[:], maskflat, ALU.mult, [bpA, bc], [bAT])
                if P2A_STAGE < 1.5:
                    continue
                pN, bpN = banks.next()
                for hl, hp, h2, pr, O in heads:
                    S.mm(pN[:, hl * 128:(hl + 1) * 128], O.aq[:, c, 0, :], O.btz[h2][:, tc], True, True, [O.buf], [bpN], signal=(hl == 3))
                Nk, bNk = Nr.next()
                S.tt("dve", Nk[:], pN[:].rearrange("p (h t) -> p h t", t=128), mLs4[:], ALU.mult, [bpN, bc], [bNk])
                if P2A_STAGE < 1.8:
                    continue
                Tt, bTt = Tr.next()
                S.tt("pool", Tt[:], AT[:, :, 0, 0, :], ident4[:], ALU.add, [bAT, bc], [bTt])
                Mk = [AT[:, hl, 0, 0, :] for hl in range(4)]
                bMk = bAT
                for lev in range(6 if P2A_STAGE >= 3 else 0):
                    last = lev == 5
                    if not last:
                        pM, bpM = banks.next()
                        for hl in range(4):
                            S.mm(pM[:, hl * 128:(hl + 1) * 128], Nk[:, hl, :], Mk[hl], True, True, [bNk, bMk], [bpM], signal=(hl == 3))
                    pN2, bpN2 = banks.next()
                    for hl in range(4):
                        S.mm(pN2[:, hl * 128:(hl + 1) * 128], Mk[hl], Nk[:, hl, :], True, True, [bNk, bMk], [bpN2], signal=(hl == 3))
                    if not last:
                        Mn, bMn = Mr.next()
                        S.copy("act", Mn[:].rearrange("p h t -> p (h t)"), pM[:], [bpM], [bMn])
                    Nn, bNn = Nr.next()
                    S.copy("act", Nn[:].rearrange("p h t -> p (h t)"), pN2[:], [bpN2], [bNn])
                    pT, bpT = banks.next()
                    for hl in range(4):
                        S.mm(pT[:, hl * 128:(hl + 1) * 128], Nn[:, hl, :], Tt[:, hl, :], True, True, [bNn, bTt], [bpT], signal=(hl == 3))
                    Tn, bTn = Tr.next()
                    S.tt("dve", Tn[:].rearrange("p h t -> p (h t)"), pT[:], Tt[:].rearrange("p h t -> p (h t)"), ALU.add, [bpT, bTt], [bTn])
                    Tt, bTt = Tn, bTn
                    Nk, bNk = Nn, bNn
                    if not last:
                        Mk = [Mn[:, hl, :] for hl in range(4)]
                        bMk = bMn
                if P2A_STAGE < 4:
                    continue
                pW, bpW = banks.next()
                for hl, hp, h2, pr, O in heads:
                    tk, btk = toks[hp]
                    S.mm(pW[:, hl * 64:(hl + 1) * 64], O.aq[:, c, 0, :], Sb[:, hp, h2, :], True, False, [O.buf, bSb[g]], [bpW], signal=False)
                    S.mm(pW[:, hl * 64:(hl + 1) * 64], AT[:, hl, 1, 0, :], tk[:, 2, 64 * h2:64 * h2 + 64], False, True, [bAT, btk], [bpW], signal=(hl == 3))
                Wb, bWb = Wbr.next()
                S.copy("act", Wb[:].rearrange("p h i -> p (h i)"), pW[:, 0:256], [bpW], [bWb])
                pU, bpU = banks.next()
                for hl in range(4):
                    S.mm(pU[:, hl * 64:(hl + 1) * 64], Tt[:, hl, :], Wb[:, hl, :], True, True, [bTt, bWb], [bpU], signal=(hl == 3))
                Ub, bUb = Ubr.next()
                S.copy("act", Ub[:].rearrange("p h i -> p (h i)"), pU[:, 0:256], [bpU], [bUb])
                pY, bpY = banks.next()
                for hl, hp, h2, pr, O in heads:
                    tk, btk = toks[hp]
                    S.mm(pY[:, hl * 64:(hl + 1) * 64], O.aq[:, c, 1, :], Sb[:, hp, h2, :], True, False, [O.buf, bSb[g]], [bpY], signal=False)
                    S.mm(pY[:, hl * 64:(hl + 1) * 64], AT[:, hl, 0, 1, :], Ub[:, hl, :], False, False, [bAT, bUb], [bpY], signal=False)
                    S.mm(pY[:, hl * 64:(hl + 1) * 64], AT[:, hl, 1, 1, :], tk[:, 2, 64 * h2:64 * h2 + 64], False, True, [bAT, btk], [bpY], signal=(hl == 3))
                S.copy("dve", yb[:, 4 * g:4 * g + 4, :].rearrange("p h i -> p (h i)"), pY[:, 0:256], [bpY], [byb])
                for hq in range(2):
                    hp = 2 * g + hq
                    tk, btk = toks[hp]
                    S.tt("pool", bon[:, 2 * hp:2 * hp + 2, :], tk[:, 2, :].rearrange("p (h i) -> p h i", i=64),
                         Dt[:, hp, :].unsqueeze(2).to_broadcast([128, 2, 64]), ALU.mult, [btk, bDt], [bbon])
                    pS, bpS = banks.next()
                    for h2 in range(2):
                        hl = 2 * hq + h2
                        S.mm(pS[:, h2 * 64:(h2 + 1) * 64], tk[:, 1, :], Ub[:, hl, :], True, False, [btk, bUb], [bpS], signal=False)
                        S.mm(pS[:, h2 * 64:(h2 + 1) * 64], tk[:, 0, :], tk[:, 2, 64 * h2:64 * h2 + 64], False, True, [btk], [bpS], signal=(h2 == 1))
                    O = outs[par][hp]
                    for h2 in range(2):
                        pr = slice(64 * h2, 64 * h2 + 64)
                        S.stt("dve", Sm[pr, hp, :], Sm[pr, hp, :], O.PC[pr, c:c + 1], pS[pr, h2 * 64:(h2 + 1) * 64], ALU.mult, ALU.add, [bpS, O.buf, bSm[hp]], [bSm[hp]])
                for z in range(2):
                    zr = slice(64 * z, 64 * z + 64)
                    S.copy("act", Sb[zr, 2 * g:2 * g + 2, z, :], Sm[zr, 2 * g:2 * g + 2, :], [bSm[2 * g], bSm[2 * g + 1]], [bSb[g]])
            k_ = 2
            for fn_ in extra:
                S.stream(k_)
                fn_()
                k_ += 1
            S.end_streams(proportional=True)
            return dict(yb=yb, byb=byb, bon=bon, bbon=bbon, par=par, tc=tc, gtok=gtok, ci=ci)

        def tail(cc):
            banks = banks_t
            yb, byb, bon, bbon, par, tc, gtok, ci = (cc[k] for k in ("yb", "byb", "bon", "bbon", "par", "tc", "gtok", "ci"))
            s8, bs8 = st8.next()
            yc, byc = ycr.next()
            sq, bsq = sqr.next()
            S.reduce("dve", s8[:, 0, :], yb[:], ALU.add, [byb], [bs8])
            S.ts("dve", s8[:, 0, :], s8[:, 0, :], 1.0 / 64, None, ALU.mult, None, [bs8], [bs8])
            S.tt("dve", yc[:], yb[:], s8[:, 0, :].unsqueeze(2).to_broadcast([128, 8, 64]), ALU.subtract, [byb, bs8], [byc])
            S.tt("pool", sq[:], yc[:], yc[:], ALU.mult, [byc], [bsq])
            S.reduce("dve", s8[:, 1, :], sq[:], ALU.add, [bsq], [bs8])
            S.ts("dve", s8[:, 1, :], s8[:, 1, :], 1.0 / 64, 64e-5, ALU.mult, ALU.add, [bs8], [bs8])
            S.act(s8[:, 1, :], s8[:, 1, :], AF.Ln, [bs8], [bs8])
            S.act(s8[:, 1, :], s8[:, 1, :], AF.Exp, [bs8], [bs8], scale=-0.5)
            S.tt("dve", yc[:], yc[:], s8[:, 1, :].unsqueeze(2).to_broadcast([128, 8, 64]), ALU.mult, [byc, bs8], [byc])
            ycf = yc[:].rearrange("p h i -> p (h i)")
            S.tt("pool", ycf, ycf, lng[:], ALU.mult, [byc, bc], [byc])
            S.tt("pool", ycf, ycf, lnb[:], ALU.add, [byc, bc], [byc])
            S.tt("pool", ycf, ycf, bon[:].rearrange("p h i -> p (h i)"), ALU.add, [byc, bbon], [byc])
            H = shared[par]
            pg, bpg = banks.next()
            S.mm(pg[:], H.sgz[:, tc], wg[:], True, True, [H.buf, bc], [bpg])
            ya, bya = yar.next()
            S.tt("dve", ya[:], ycf, pg[:], ALU.mult, [byc, bpg], [bya])
            pb_, bpb = banks.next()
            pbv = pb_[:].bitcast(BF16)
            for j in range(4):
                S.tr(pbv[:, j * 128:(j + 1) * 128], ya[:, j * 128:(j + 1) * 128], identb[:], [bya, bc], [bpb], signal=(j == 3))
            yaT, byaT = yaTr.next()
            S.copy("act", yaT[:].rearrange("p j t -> p (j t)"), pbv[:, 0:512], [bpb], [byaT])
            S.dma("sp", D["YAT"][4 * half:4 * half + 4, :, gtok].rearrange("j p t -> p j t"), yaT[:], [byaT], [cx.b_YAT[ci]])

        print("phase2a sbuf remaining", nc.sbuf_bytes_remaining)
        pre_shared(0)
        for hp in range(4):
            pre(0, hp)
        prev = None
        hd = head(0, 0)
        nxt = {}
        for T in range(16):
            for c in range(4):
                extra = []
                if prev is not None:
                    extra.append(lambda p_=prev: tail(p_))
                ci = T * 4 + c
                has_next = ci + 1 < 64
                Tn, cn = divmod(ci + 1, 4)

                def prestream(T_=T, c_=c, Tn_=Tn, cn_=cn, has_next_=has_next):
                    if T_ + 1 < 16:
                        if c_ == 1:
                            pre_shared(T_ + 1)
                            pre(T_ + 1, 0)
                            pre(T_ + 1, 1)
                        elif c_ >= 2:
                            pre(T_ + 1, c_)
                    if has_next_:
                        nxt["hd"] = head(Tn_, cn_)

                extra.append(prestream)
                prev = chunk(T, c, extra, hd)
                hd = nxt.get("hd")
        tail(prev)
    S.barrier()


def phase2b(cx):
    nc, S, I, D = cx.nc, cx.S, cx.inp, cx.scr
    with ExitStack() as es:
        sb, ps = _alloc(nc, es)
        _bk = [ps(f"p3_bank{i}", [128, 512], F32) for i in range(8)]
        bkX = [(_bk[2 * g], Buf()) for g in range(4)]
        bkY = [(_bk[2 * g + 1], Buf()) for g in range(4)]
        bc = Buf()
        identb = sb("b_identb", [128, 128], BF16)
        S.copy("pool", identb[:], cx.identf[:], [cx.b_const], [bc])
        maskBD = sb("b_maskBD", [128, 128], F32)
        mLs = sb("b_mLs", [128, 128], F32)
        sameblk = sb("b_sameblk", [128, 128], F32)
        cones = [sb(f"b_cones{i}", [128, 128], F32) for i in range(2)]
        mUi8 = sb("b_mUi8", [128, 8, 128], F32)
        S.memset("pool", maskBD[:], 1.0, [bc])
        S.asel(maskBD[:], maskBD[:], [[1, 128]], ALU.is_ge, 0.0, 0, -1, [bc], [bc])
        for x in range(8):
            S.copy("pool", mUi8[:, x, :], maskBD[:], [bc], [bc])
        S.memset("pool", maskBD[0:64, 64:128], 0.0, [bc])
        S.memset("pool", mLs[:], 1.0, [bc])
        S.asel(mLs[:], mLs[:], [[-1, 128]], ALU.is_gt, 0.0, 0, 1, [bc], [bc])
        S.memset("pool", sameblk[:], 0.0, [bc])
        S.memset("pool", sameblk[0:64, 0:64], 1.0, [bc])
        S.memset("pool", sameblk[64:128, 64:128], 1.0, [bc])
        for i in range(2):
            S.memset("pool", cones[i][:], 0.0, [bc])
            S.memset("pool", cones[i][64 * i:64 * i + 64, :], 1.0, [bc])
        A_b = sb("b_A", [128, 32], F32)
        dsk = sb("b_dsk", [128, 32], F32)
        normg = sb("b_normg", [128, 2048], F32)
        S.dma("sp", A_b[:], I["alog_b"], writes=[bc])
        S.dma("sp", dsk[:], I["dskip_b"], writes=[bc])
        S.dma("sp", normg[:], I["normg_b"], writes=[bc])
        S.act(A_b[:], A_b[:], AF.Exp, [bc], [bc])
        S.ts("dve", A_b[:], A_b[:], -1.0, None, ALU.mult, None, [bc], [bc])
        Hm = sb("b_Hm", [128, 4, 512], F32)
        bHm = [Buf() for _ in range(4)]
        Hs = [[sb(f"b_Hs{g}{k}", [128, 512], BF16) for k in range(2)] for g in range(4)]
        bHs = [[Buf(), Buf()] for g in range(4)]
        S.memset("pool", Hm[:], 0.0, bHm)
        zt = [[sb(f"b_z{g}{k}", [128, 512], BF16) for k in range(2)] for g in range(4)]
        bzt = [[Buf(), Buf()] for g in range(4)]
        Cz = [[sb(f"b_Cz{g}{k}", [128, 128], BF16) for k in range(2)] for g in range(4)]
        bCz = [[Buf(), Buf()] for g in range(4)]
        for g in range(4):
            for k in range(2):
                S.memset("pool", Hs[g][k][:], 0.0, [bHs[g][k]])
                S.memset("pool", zt[g][k][:], 0.0, [bzt[g][k]])
                S.memset("pool", Cz[g][k][:], 0.0, [bCz[g][k]])

        def mk(name, shape, dt, n):
            return [Ring([sb(f"b_{name}{g}_{i}", shape, dt) for i in range(n)]) for g in range(4)]

        xsFr = mk("xsF", [128, 4, 128], BF16, 2)
        BTr = mk("BT", [128, 128], BF16, 2)
        CTr = mk("CT", [128, 128], BF16, 2)
        szr = mk("sz", [128, 512], BF16, 2)
        xsr = mk("xs", [128, 512], BF16, 1)
        Btmr = mk("Btm", [128, 128], BF16, 1)
        smr = mk("sm", [128, 48], F32, 1)
        xdtr = mk("xdt", [128, 512], BF16, 1)
        R8r = mk("R8", [128, 8, 128], F32, 1)
        Dmr = mk("Dm", [128, 8, 128], BF16, 1)
        MTr = mk("MT", [128, 8, 128], BF16, 1)
        cbr = mk("cb", [128, 128], BF16, 1)
        tr_ = mk("t", [128, 512], F32, 1)
        t0r = mk("t0", [128, 512], F32, 1)
        t2r = mk("t2", [128, 512], F32, 1)
        ssr = mk("ss", [128, 2], F32, 1)
        ybr = mk("yb", [128, 512], BF16, 1)
        ybTr = mk("ybT", [128, 4, 128], BF16, 1)
        dtr = Ring([sb(f"b_dt{i}", [128, 32], F32) for i in range(3)])
        adtr = Ring([sb(f"b_adt{i}", [128, 32], F32) for i in range(2)])
        print("phase2b sbuf remaining", nc.sbuf_bytes_remaining)

        def h3(ap):
            return ap.rearrange("p (h i) -> p h i", i=64)

        for i in range(NT):
            tok = slice(i * 128, (i + 1) * 128)
            tq = i // 4
            dt_t, bdt = dtr.next()
            S.dma("sp", dt_t[:], D["DT"][tok, :], [cx.b_DT[i]], [bdt])
            adt, badt = adtr.next()
            S.tt("dve", adt[:], dt_t[:], A_b[:], ALU.mult, [bdt, bc], [badt])
            S.begin_streams(4)
            for gg in range(4):
                S.stream(gg)
                g = gg
                pX, bpX = bkX[gg]
                pY, bpY = bkY[gg]
                xsF, bxsF = xsFr[gg].next()
                S.dma("sp", xsF[:], D["XBC"][4 * gg:4 * gg + 4, :, tok].rearrange("j p t -> p j t"), [cx.b_XBC[4 * gg + j][tq] for j in range(4)], [bxsF])
                BT, bBT = BTr[gg].next()
                S.dma("sp", BT[:], D["XBC"][16 + gg, :, tok], [cx.b_XBC[16 + gg][tq]], [bBT])
                CT, bCT = CTr[gg].next()
                S.dma("sp", CT[:], D["XBC"][20 + gg, :, tok], [cx.b_XBC[20 + gg][tq]], [bCT])
                sz, bsz = szr[gg].next()
                S.dma("sp", sz[:], D["SZ"][tok, gg * 512:(gg + 1) * 512], [cx.b_SZ[i]], [bsz])
                gs = slice(8 * gg, 8 * gg + 8)
                pbv = pX[:].bitcast(BF16)
                for j in range(4):
                    S.tr(pbv[:, j * 128:(j + 1) * 128], xsF[:, j, :], identb[:], [bxsF, bc], [bpX], signal=False)
                S.tr(pbv[:, 512:640], BT[:], identb[:], [bBT, bc], [bpX])
                xs, bxs = xsr[gg].next()
                S.copy("act", xs[:], pbv[:, 0:512], [bpX], [bxs])
                Btm, bBtm = Btmr[gg].next()
                S.copy("act", Btm[:], pbv[:, 512:640], [bpX], [bBtm])
                S.mm(pY[:, 0:8], maskBD[:], adt[:, gs], True, True, [bc, badt], [bpY], signal=False)
                S.mm(pY[:, 8:16], sameblk[:], adt[:, gs], True, True, [bc, badt], [bpY], signal=False)
                S.mm(pY[:, 16:24], cones[0][:], adt[:, gs], True, True, [bc, badt], [bpY], signal=False)
                S.mm(pY[:, 24:32], cones[1][:], adt[:, gs], True, True, [bc, badt], [bpY])
                sm, bsm = smr[gg].next()
                S.copy("dve", sm[:, 0:32], pY[:, 0:32], [bpY], [bsm])
                S.tt("dve", sm[:, 40:48], sm[:, 8:16], sm[:, 0:8], ALU.subtract, [bsm], [bsm])
                S.act(sm[:, 32:40], sm[:, 0:8], AF.Exp, [bsm], [bsm])
                S.act(sm[:, 40:48], sm[:, 40:48], AF.Exp, [bsm], [bsm])
                S.act(sm[:, 16:32], sm[:, 16:32], AF.Exp, [bsm], [bsm])
                xdt, bxdt = xdtr[gg].next()
                S.tt("dve", h3(xdt[:]), h3(xs[:]), dt_t[:, gs].unsqueeze(2).to_broadcast([128, 8, 64]), ALU.mult, [bxs, bdt], [bxdt])
                for k in range(2):
                    kr = slice(64 * k, 64 * k + 64)
                    S.tt("pool", h3(zt[gg][k][kr, :]), h3(xdt[kr, :]), sm[kr, 40:48].unsqueeze(2).to_broadcast([64, 8, 64]), ALU.mult, [bxdt, bsm], [bzt[gg][k]])
                    S.copy("pool", Cz[gg][k][:, kr], CT[:, kr], [bCT], [bCz[gg][k]])
                R8, bR8 = R8r[gg].next()
                S.tt("dve", R8[:], mUi8[:], adt[:, gs].unsqueeze(2).to_broadcast([128, 8, 128]), ALU.mult, [bc, badt], [bR8])
                Dm, bDm = Dmr[gg].next()
                for hf, (pseg, bpseg) in enumerate(((pX, bpX), (pY, bpY))):
                    S.mm(pseg[:], mLs[:], R8[:, 4 * hf:4 * hf + 4, :].rearrange("p h t -> p (h t)"), True, True, [bc, bR8], [bpseg])
                    S.act(Dm[:, 4 * hf:4 * hf + 4, :].rearrange("p h t -> p (h t)"), pseg[:], AF.Exp, [bpseg], [bDm])
                S.mm(pX[:, 0:128], BT[:], CT[:], True, True, [bBT, bCT], [bpX])
                cb, bcb = cbr[gg].next()
                S.tt("dve", cb[:], pX[:, 0:128], maskBD[:], ALU.mult, [bpX, bc], [bcb])
                MT, bMT = MTr[gg].next()
                S.tt("pool", MT[:], Dm[:], cb[:].unsqueeze(1).to_broadcast([128, 8, 128]), ALU.mult, [bDm, bcb], [bMT])
                for h in range(8):
                    S.mm(pY[:, h * 64:(h + 1) * 64], MT[:, h, :], xdt[:, h * 64:(h + 1) * 64], True, True, [bMT, bxdt], [bpY], signal=(h == 7))
                t0, bt0 = t0r[gg].next()
                S.copy("dve", t0[:], pY[:], [bpY], [bt0])
                S.mm(pX[:], Cz[gg][0][:], Hs[gg][0][:], True, False, [bCz[gg][0], bHs[gg][0]], [bpX], signal=False)
                S.mm(pY[:], Btm[:], zt[gg][0][:], True, True, [bBtm, bzt[gg][0]], [bpY])
                Hg = Hm[:, gg, :]
                S.tt("dve", h3(Hg), h3(Hg), sm[:, 16:24].unsqueeze(2).to_broadcast([128, 8, 64]), ALU.mult, [bHm[gg], bsm], [bHm[gg]])
                S.tt("dve", Hg, Hg, pY[:], ALU.add, [bHm[gg], bpY], [bHm[gg]])
                S.copy("act", Hs[gg][1][:], Hg, [bHm[gg]], [bHs[gg][1]])
                S.mm(pX[:], Cz[gg][1][:], Hs[gg][1][:], False, True, [bCz[gg][1], bHs[gg][1]], [bpX])
                S.mm(pY[:], Btm[:], zt[gg][1][:], True, True, [bBtm, bzt[gg][1]], [bpY])
                S.tt("dve", h3(Hg), h3(Hg), sm[:, 24:32].unsqueeze(2).to_broadcast([128, 8, 64]), ALU.mult, [bHm[gg], bsm], [bHm[gg]])
                S.tt("dve", Hg, Hg, pY[:], ALU.add, [bHm[gg], bpY], [bHm[gg]])
                S.copy("act", Hs[gg][0][:], Hg, [bHm[gg]], [bHs[gg][0]])
                t, bt = tr_[gg].next()
                S.tt("dve", h3(t[:]), h3(pX[:]), sm[:, 32:40].unsqueeze(2).to_broadcast([128, 8, 64]), ALU.mult, [bpX, bsm], [bt])
                S.tt("pool", t[:], t[:], t0[:], ALU.add, [bt, bt0], [bt])
                t2, bt2 = t2r[gg].next()
                S.tt("pool", h3(t2[:]), h3(xs[:]), dsk[:, gs].unsqueeze(2).to_broadcast([128, 8, 64]), ALU.mult, [bxs, bc], [bt2])
                S.tt("pool", t[:], t[:], t2[:], ALU.add, [bt, bt2], [bt])
                S.tt("pool", t[:], t[:], sz[:], ALU.mult, [bt, bsz], [bt])
                ss, bss = ssr[gg].next()
                S.memset("pool", ss[:], 0.0, [bss])
                S.act(t2[:], t[:], AF.Square, [bt], [bt2, bss], accum_out=ss[:, 0:1])
                S.ts("dve", ss[:, 1:2], ss[:, 0:1], 1.0 / 512, 1e-6, ALU.mult, ALU.add, [bss], [bss])
                S.act(ss[:, 1:2], ss[:, 1:2], AF.Ln, [bss], [bss])
                S.act(ss[:, 1:2], ss[:, 1:2], AF.Exp, [bss], [bss], scale=-0.5)
                yb, byb = ybr[gg].next()
                S.stt("dve", yb[:], t[:], ss[:, 1:2], normg[:, gg * 512:(gg + 1) * 512], ALU.mult, ALU.mult, [bt, bss, bc], [byb])
                pbv2 = pY[:].bitcast(BF16)
                for j in range(4):
                    S.tr(pbv2[:, j * 128:(j + 1) * 128], yb[:, j * 128:(j + 1) * 128], identb[:], [byb, bc], [bpY], signal=(j == 3))
                ybT, bybT = ybTr[gg].next()
                S.copy("act", ybT[:].rearrange("p j t -> p (j t)"), pbv2[:, 0:512], [bpY], [bybT])
                S.dma("sp", D["YBT"][4 * gg:4 * gg + 4, :, tok].rearrange("j p t -> p j t"), ybT[:], [bybT], [cx.b_YBT[i]])
            S.end_streams()
    S.barrier()


def phase2c(cx):
    nc, S, I, D = cx.nc, cx.S, cx.inp, cx.scr
    with ExitStack() as es:
        sb, ps = _alloc(nc, es)
        banks = Ring([ps(f"c_bank{i}", [128, 512], F32) for i in range(8)])
        bc = Buf()
        wupA = sb("c_wupA", [128, 8, 1024], BF16)
        wupB = sb("c_wupB", [128, 16, 1024], BF16)
        wout = sb("c_wout", [128, 8, 1024], BF16)
        S.dma("pool", wupA[:], I["w_up_rwkv"].rearrange("(k p) c -> p k c", p=128), writes=[bc])
        for q in range(2):
            S.dma("pool", wupB[:, 8 * q:8 * q + 8, :], I["w_up_ssd"][1024 * q:1024 * q + 1024, :].rearrange("(k p) c -> p k c", p=128), writes=[bc])
        S.dma("pool", wout[:], I["w_out"].rearrange("(k p) c -> p k c", p=128), writes=[bc])
        yar = Ring([sb(f"c_ya{i}", [128, 8, 512], BF16) for i in range(2)])
        ybr = Ring([sb(f"c_yb{i}", [128, 16, 512], BF16) for i in range(2)])
        gar = Ring([sb(f"c_ga{i}", [128, 16, 512], BF16) for i in range(2)])
        mgr = Ring([sb(f"c_mg{i}", [128, 8, 512], BF16) for i in range(1)])
        t1r = Ring([sb(f"c_t1{i}", [128, 512], F32) for i in range(2)])
        t2r = Ring([sb(f"c_t2{i}", [128, 512], F32) for i in range(2)])
        xr = Ring([sb(f"c_x{i}", [128, 1024], F32) for i in range(2)])
        xnr = Ring([sb(f"c_xn{i}", [128, 1024], F32) for i in range(2)])
        print("phase2c sbuf remaining", nc.sbuf_bytes_remaining)
        for T in range(16):
            tok = slice(T * 512, (T + 1) * 512)
            ya, bya = yar.next()
            S.dma("sp", ya[:], D["YAT"][:, :, tok].rearrange("j p t -> p j t"), cx.b_YAT[4 * T:4 * T + 4], [bya])
            yb, byb = ybr.next()
            S.dma("sp", yb[:], D["YBT"][:, :, tok].rearrange("j p t -> p j t"), cx.b_YBT[4 * T:4 * T + 4], [byb])
            ga, bga = gar.next()
            S.dma("sp", ga[:], D["GA"][:, :, tok].rearrange("j p t -> p j t"), [cx.b_GA[j][T] for j in range(16)], [bga])
            mg, bmg = mgr.next()
            for ob in range(8):
                oc = slice(ob * 128, (ob + 1) * 128)
                pA, bpA = banks.next()
                for k in range(8):
                    S.mm(pA[:], wupA[:, k, oc], ya[:, k, :], k == 0, k == 7, [bc, bya], [bpA], signal=(k == 7))
                pB, bpB = banks.next()
                for k in range(16):
                    S.mm(pB[:], wupB[:, k, oc], yb[:, k, :], k == 0, k == 15, [bc, byb], [bpB], signal=(k == 15))
                t1, bt1 = t1r.next()
                S.tt("dve", t1[:], pA[:], ga[:, ob, :], ALU.mult, [bpA, bga], [bt1])
                t2, bt2 = t2r.next()
                S.tt("dve", t2[:], pB[:], ga[:, 8 + ob, :], ALU.mult, [bpB, bga], [bt2])
                S.tt("pool", mg[:, ob, :], t1[:], t2[:], ALU.add, [bt1, bt2], [bmg])
            for sub in range(4):
                ti = 4 * T + sub
                rows = slice(ti * 128, (ti + 1) * 128)
                x_t, bx = xr.next()
                S.dma("sp", x_t[:], I["xb"][rows, :], writes=[bx])
                xn, bxn = xnr.next()
                for hf in range(2):
                    hs = slice(hf * 512, (hf + 1) * 512)
                    pD, bpD = banks.next()
                    for k in range(8):
                        S.mm(pD[:], mg[:, k, sub * 128:(sub + 1) * 128], wout[:, k, hs], k == 0, k == 7, [bmg, bc], [bpD], signal=(k == 7))
                    S.tt("dve", xn[:, hs], pD[:], x_t[:, hs], ALU.add, [bpD, bx], [bxn])
                S.dma("sp", D["XN"][rows, :], xn[:], [bxn], [cx.b_XN[ti]])
    S.barrier()


def phase3(cx):
    nc, S, I, D = cx.nc, cx.S, cx.inp, cx.scr
    with ExitStack() as es:
        sb, ps = _alloc(nc, es)
        _bk = [ps(f"m_bank{i}", [128, 512], F32) for i in range(8)]
        banks = Ring(_bk)
        banks_s = [Ring(_bk[0:4]), Ring(_bk[4:8])]
        bc = Buf()
        msel = sb("m_msel", [128, 2], F32)
        fgT = sb("m_fgT", [128, 8], F32)
        finb = sb("m_finb", [128, 1024], F32)
        wr = sb("m_wr", [128, 8, 36], F32)
        rbb = sb("m_rbb", [128, 36], F32)
        S.dma("sp", msel[:], I["msel"], writes=[bc])
        S.dma("sp", fgT[:], I["ffn_gT"], writes=[bc])
        S.dma("sp", finb[:], I["fin_b"], writes=[bc])
        S.dma("sp", wr[:], I["w_router"].rearrange("(k p) c -> p k c", p=128), writes=[bc])
        S.dma("sp", rbb[:], I["rb_b"], writes=[bc])
        acc = sb("m_acc", [128, 16, 1024], F32)
        b_acc = [Buf() for _ in range(16)]
        xnT = sb("m_xnT", [128, 8, 2048], BF16)
        b_xnT = [Buf() for _ in range(16)]
        coef = sb("m_coef", [128, 16, 32], F32)
        b_coef = [Buf() for _ in range(16)]
        ldr_s = [Ring([sb(f"m_ld{q}{i}", [128, 1024], F32) for i in range(1)]) for q in range(2)]
        xsr_s = [Ring([sb(f"m_xs{q}{i}", [128, 1024], F32) for i in range(1)]) for q in range(2)]
        ssr_s = [Ring([sb(f"m_ss{q}{i}", [128, 2], F32) for i in range(2)]) for q in range(2)]
        xrr_s = [Ring([sb(f"m_xr{q}{i}", [128, 8, 128], F32) for i in range(1)]) for q in range(2)]
        rtr_s = [Ring([sb(f"m_rt{q}{i}", [128, 128], F32) for i in range(2)]) for q in range(2)]
        xsr, ssr = xsr_s[0], ssr_s[0]
        wgr = Ring([sb(f"m_wg{i}", [128, 8, 512], BF16) for i in range(2)])
        wur = Ring([sb(f"m_wu{i}", [128, 8, 512], BF16) for i in range(2)])
        wdr = Ring([sb(f"m_wd{i}", [128, 4, 1024], BF16) for i in range(2)])
        hidr = Ring([sb(f"m_hid{i}", [128, 4, 512], BF16) for i in range(2)])
        sgr = Ring([sb(f"m_sg{i}", [128, 512], F32) for i in range(2)])
        outr = xsr_s[1]
        print("phase3 sbuf remaining", nc.sbuf_bytes_remaining)
        for p in range(2):
            for i in range(16):
                if i % 2 == 0:
                    S.begin_streams(2)
                q_ = i % 2
                S.stream(q_)
                banks = banks_s[q_]
                ldr, xsr, ssr, xrr, rtr = ldr_s[q_], xsr_s[q_], ssr_s[q_], xrr_s[q_], rtr_s[q_]
                lt = p * 16 + i
                A_t, bA = ldr.next()
                S.dma("sp", A_t[:], D["XN"][lt * 128:(lt + 1) * 128, :], [cx.b_XN[lt]], [bA])
                B_t, bB = xsr.next()
                S.dma("sp", B_t[:], D["XN"][4096 + lt * 128:4096 + (lt + 1) * 128, :], [cx.b_XN[32 + lt]], [bB])
                S.ts("dve", acc[:, i, :], A_t[:], msel[:, 0:1], None, ALU.mult, None, [bA, bc], [b_acc[i]])
                S.stt("dve", acc[:, i, :], B_t[:], msel[:, 1:2], acc[:, i, :], ALU.mult, ALU.add, [bB, bc, b_acc[i]], [b_acc[i]])
                xs_t, bxs = xsr.next()
                ss, bss = ssr.next()
                S.memset("pool", ss[:], 0.0, [bss])
                S.act(xs_t[:], acc[:, i, :], AF.Square, [b_acc[i]], [bxs, bss], accum_out=ss[:, 0:1])
                S.ts("dve", ss[:, 1:2], ss[:, 0:1], 1.0 / DM, 1e-6, ALU.mult, ALU.add, [bss], [bss])
                S.act(ss[:, 1:2], ss[:, 1:2], AF.Sqrt, [bss], [bss])
                S.op("dve", lambda e, o=ss[:, 1:2]: e.reciprocal(out=o, in_=o), [bss], [bss])
                S.act(xs_t[:], acc[:, i, :], AF.Copy, [b_acc[i], bss], [bxs], scale=ss[:, 1:2])
                xr_, bxr = xrr.next()
                for hf in range(2):
                    pp, bp = banks.next()
                    for k in range(4):
                        kk = hf * 4 + k
                        S.tr(pp[:, k * 128:(k + 1) * 128], xs_t[:, kk * 128:(kk + 1) * 128], cx.identf[:], [bxs, cx.b_const], [bp], signal=(k == 3))
                    gb = fgT[:, hf * 4:hf * 4 + 4].unsqueeze(2).to_broadcast([128, 4, 128])
                    pv3 = pp[:].rearrange("p (k t) -> p k t", t=128)
                    S.tt("dve", xnT[:, hf * 4:hf * 4 + 4, i * 128:(i + 1) * 128], pv3, gb, ALU.mult, [bp, bc], [b_xnT[i]])
                    S.tt("dve", xr_[:, hf * 4:hf * 4 + 4, :], pv3, gb, ALU.mult, [bp, bc], [bxr])
                pr, bpr = banks.next()
                for k in range(8):
                    S.mm(pr[:, 0:36], xr_[:, k, :], wr[:, k, :], k == 0, k == 7, [bxr, bc], [bpr], signal=(k == 7))
                rt, brt = rtr.next()
                lg = rt[:, 0:36]
                gm = rt[:, 36:40]
                oh = rt[:, 40:44]
                pen = rt[:, 44:48]
                me = rt[:, 48:80]
                mx8 = rt[:, 80:88]
                q = rt[:, 88:96]
                c1 = rt[:, 96:128]
                S.tt("dve", lg, pr[:, 0:36], rbb[:], ALU.add, [bpr, bc], [brt])
                S.reduce("dve", gm[:, 0:1], lg[:, 0:4], ALU.max, [brt], [brt])
                S.ts("dve", gm[:, 1:2], gm[:, 0:1], -1.0, None, ALU.mult, None, [brt], [brt])
                S.ts("dve", oh, lg[:, 0:4], gm[:, 0:1], None, ALU.is_equal, None, [brt], [brt])
                S.memset("pool", gm[:, 2:3], 0.0, [brt])
                S.act(pen, lg[:, 0:4], AF.Exp, [brt], [brt], bias=gm[:, 1:2], accum_out=gm[:, 2:3])
                S.op("dve", lambda e, o=gm[:, 3:4], i_=gm[:, 2:3]: e.reciprocal(out=o, in_=i_), [brt], [brt])
                S.ts("dve", pen, oh, -1.0, 1e30, ALU.add, ALU.mult, [brt], [brt])
                S.tt("dve", me.rearrange("p (g e) -> p g e", e=8), lg[:, 4:36].rearrange("p (g e) -> p g e", e=8),
                     pen.unsqueeze(2).to_broadcast([128, 4, 8]), ALU.add, [brt], [brt])
                S.op("dve", lambda e, o=mx8, i_=me: e.max(out=o, in_=i_), [brt], [brt])
                S.tt("dve", q[:, 0:1], mx8[:, 1:2], mx8[:, 0:1], ALU.subtract, [brt], [brt])
                S.act(q[:, 1:2], q[:, 0:1], AF.Exp, [brt], [brt])
                S.ts("dve", q[:, 2:3], q[:, 1:2], 1.0, None, ALU.add, None, [brt], [brt])
                S.op("dve", lambda e, o=q[:, 2:3]: e.reciprocal(out=o, in_=o), [brt], [brt])
                S.tt("dve", q[:, 3:4], q[:, 2:3], gm[:, 3:4], ALU.mult, [brt], [brt])
                S.tt("dve", q[:, 4:5], q[:, 3:4], q[:, 1:2], ALU.mult, [brt], [brt])
                S.ts("dve", c1, me, mx8[:, 0:1], q[:, 3:4], ALU.is_equal, ALU.mult, [brt], [brt])
                S.ts("dve", me, me, mx8[:, 1:2], q[:, 4:5], ALU.is_equal, ALU.mult, [brt], [brt])
                S.tt("dve", coef[:, i, :], c1, me, ALU.add, [brt], [b_coef[i]])
                if i % 2 == 1:
                    S.end_streams()
            banks = Ring(_bk)
            xsr, ssr = xsr_s[0], ssr_s[0]
            for e_ in range(32):
                wg_, bwg = wgr.next()
                S.dma("pool", wg_[:], I["w_exp_gate"][e_].rearrange("(k p) c -> p k c", p=128), writes=[bwg])
                wu_, bwu = wur.next()
                S.dma("pool", wu_[:], I["w_exp_up"][e_].rearrange("(k p) c -> p k c", p=128), writes=[bwu])
                wd_, bwd = wdr.next()
                S.dma("pool", wd_[:], I["w_exp_down"][e_].rearrange("(k p) c -> p k c", p=128), writes=[bwd])
                for tt_ in range(4):
                    tk = slice(tt_ * 512, (tt_ + 1) * 512)
                    hid, bhid = hidr.next()
                    for hb in range(4):
                        hc = slice(hb * 128, (hb + 1) * 128)
                        pG, bpG = banks.next()
                        for k in range(8):
                            S.mm(pG[:], wg_[:, k, hc], xnT[:, k, tk], k == 0, k == 7, [bwg] + b_xnT[4 * tt_:4 * tt_ + 4], [bpG], signal=(k == 7))
                        pU, bpU = banks.next()
                        for k in range(8):
                            S.mm(pU[:], wu_[:, k, hc], xnT[:, k, tk], k == 0, k == 7, [bwu] + b_xnT[4 * tt_:4 * tt_ + 4], [bpU], signal=(k == 7))
                        sg, bsg = sgr.next()
                        S.act(sg[:], pG[:], AF.Silu, [bpG], [bsg])
                        S.tt("dve", hid[:, hb, :], sg[:], pU[:], ALU.mult, [bsg, bpU], [bhid])
                    for sub in range(4):
                        i = tt_ * 4 + sub
                        for hf in range(2):
                            hs = slice(hf * 512, (hf + 1) * 512)
                            pD, bpD = banks.next()
                            for kb in range(4):
                                S.mm(pD[:], hid[:, kb, sub * 128:(sub + 1) * 128], wd_[:, kb, hs], kb == 0, kb == 3, [bhid, bwd], [bpD], signal=(kb == 3))
                            S.stt("dve", acc[:, i, hs], pD[:], coef[:, i, e_:e_ + 1], acc[:, i, hs], ALU.mult, ALU.add, [bpD, b_coef[i], b_acc[i]], [b_acc[i]])
            for i in range(16):
                lt = p * 16 + i
                xs_t, bxs = xsr.next()
                ss, bss = ssr.next()
                S.memset("pool", ss[:], 0.0, [bss])
                S.act(xs_t[:], acc[:, i, :], AF.Square, [b_acc[i]], [bxs, bss], accum_out=ss[:, 0:1])
                S.ts("dve", ss[:, 1:2], ss[:, 0:1], 1.0 / DM, 1e-6, ALU.mult, ALU.add, [bss], [bss])
                S.act(ss[:, 1:2], ss[:, 1:2], AF.Sqrt, [bss], [bss])
                S.op("dve", lambda e, o=ss[:, 1:2]: e.reciprocal(out=o, in_=o), [bss], [bss])
                o_t, bo = outr.next()
                S.stt("dve", o_t[:], acc[:, i, :], ss[:, 1:2], finb[:], ALU.mult, ALU.mult, [b_acc[i], bss, bc], [bo])
                S.dma("sp", cx.out[lt * 128:(lt + 1) * 128, :], o_t[:], [bo], [cx.b_out])


def build_program():
    nc = bass.Bass("TRN2", target_bir_lowering=False)
    cx = Ctx()
    cx.nc = nc
    cx.inp = {}
    cx.scr = {}
    cx.outs = {}

    def inp(name, shape):
        cx.inp[name] = nc.dram_tensor(name, shape, F32, kind="ExternalInput").ap()

    def scr(name, shape, dt):
        kind = "ExternalOutput" if DEBUG else "Internal"
        cx.scr[name] = nc.dram_tensor(name, shape, dt, kind=kind).ap()

    inp("xb", [SEQ, DM])
    inp("attn_gT", [128, 8])
    inp("pv_fm", [128, NFM, 5])
    inp("dt_bias_b", [128, 32])
    inp("wfm", [DM, NFM * 128])
    inp("wz", [DM, 2048])
    inp("wdt", [DM, 32])
    inp("pv_rw", [128, 8, 5])
    inp("wlora_w", [128, 1024])
    inp("wlora_a", [128, 1024])
    inp("wg", [128, 1024])
    inp("lng_b", [128, 1024])
    inp("lnb_b", [128, 1024])
    inp("alog_b", [128, 32])
    inp("dskip_b", [128, 32])
    inp("normg_b", [128, 2048])
    inp("w_up_rwkv", [1024, 1024])
    inp("w_up_ssd", [2048, 1024])
    inp("w_out", [1024, 1024])
    inp("msel", [128, 2])
    inp("ffn_gT", [128, 8])
    inp("fin_b", [128, 1024])
    inp("w_router", [1024, 36])
    inp("rb_b", [128, 36])
    inp("w_exp_gate", [32, 1024, 512])
    inp("w_exp_up", [32, 1024, 512])
    inp("w_exp_down", [32, 512, 1024])
    scr("XN", [SEQ, 1024], F32)
    cx.b_XN = [Buf() for _ in range(NT)]
    cx.out = nc.dram_tensor("out", [4096, 1024], F32, kind="ExternalOutput").ap()
    cx.b_out = Buf()
    scr("YBT", [16, 128, SEQ], BF16)
    cx.b_YBT = [Buf() for _ in range(NT)]
    scr("YAT", [8, 128, SEQ], BF16)
    cx.b_YAT = [Buf() for _ in range(NT)]
    scr("RW", [26, 128, SEQ], F32)
    scr("XBC", [24, 128, SEQ], BF16)
    scr("GA", [16, 128, SEQ], BF16)
    scr("SZ", [SEQ, 2048], BF16)
    scr("DT", [SEQ, 32], F32)
    cx.b_RW = [[Buf() for _ in range(16)] for _ in range(26)]
    cx.b_XBC = [[Buf() for _ in range(16)] for _ in range(24)]
    cx.b_GA = [[Buf() for _ in range(16)] for _ in range(16)]
    cx.b_SZ = [Buf() for _ in range(NT)]
    cx.b_DT = [Buf() for _ in range(NT)]

    with ExitStack() as es:
        S = Sched(nc, es)
        cx.S = S
        sb, ps = _alloc(nc, es)
        cx.b_const = Buf()
        cx.identf = sb("identf", [128, 128], F32)
        S.memset("pool", cx.identf[:], 1.0, [cx.b_const])
        S.asel(cx.identf[:], cx.identf[:], [[-1, 128]], ALU.is_equal, 0.0, 0, 1, [cx.b_const], [cx.b_const])
        phase1(cx)
        for half in range(2):
            if STOP_AFTER >= 2 and not SKIP_2A:
                phase2a(cx, half)
        if STOP_AFTER >= 3:
            phase2b(cx)
        if STOP_AFTER >= 4:
            phase2c(cx)
        if STOP_AFTER >= 5:
            phase3(cx)
        outs = []
        for name in ("RW", "XBC", "GA", "SZ", "DT"):
            pass
        allb = [cx.b_out] + cx.b_XN + cx.b_YAT + cx.b_YBT + [b for l in cx.b_RW for b in l] + [b for l in cx.b_XBC for b in l] + [b for l in cx.b_GA for b in l] + cx.b_SZ + cx.b_DT
        S._waits("sp", allb, allb)
        S.build()
        print("instructions:", S.ninstr, "counts:", S.cnt, S.dn)
    return nc


def host_prep(inputs, c):
    b, hh = c // 2, c % 2
    f = lambda a: np.ascontiguousarray(a, dtype=np.float32)
    w_in = inputs["w_in"][0]
    SS0, G0 = 3328, 8480
    cols = np.concatenate([np.arange(0, 3328), np.arange(SS0 + 2048, SS0 + 5120), np.arange(G0, G0 + 2048)])
    assert cols.size == NFM * 128
    m = {}
    m["xb"] = f(inputs["x"][b])
    m["attn_gT"] = f(inputs["attn_norm_g"][0].reshape(8, 128).T)
    m["wfm"] = f(w_in[:, cols])
    m["wz"] = f(w_in[:, SS0:SS0 + 2048])
    m["wdt"] = f(w_in[:, SS0 + 5120:SS0 + 5152])
    pv = np.zeros((NFM * 128, 5), np.float32)
    pv[0:3328, 0] = inputs["rwkv_mu"][0]
    pv[3328:6400, 0:4] = inputs["ssd_conv_w"][0].T
    pv[3328:6400, 4] = inputs["ssd_conv_b"][0]
    pv[6400:, 4] = inputs["b_gate"][0]
    m["pv_fm"] = f(pv.reshape(NFM, 128, 5).transpose(1, 0, 2))
    m["dt_bias_b"] = f(np.broadcast_to(inputs["ssd_dt_bias"][0], (128, 32)))
    pvr = np.stack([inputs["rwkv_w0"][0], inputs["rwkv_a0"][0], inputs["rwkv_k_k"][0],
                    inputs["rwkv_k_a"][0], inputs["rwkv_r_k"][0].reshape(-1)], axis=1)
    m["pv_rw"] = f(pvr.reshape(8, 128, 5).transpose(1, 0, 2))
    zz = np.zeros((64, 1024), np.float32)
    m["wlora_w"] = f(np.vstack([inputs["rwkv_w_decay"][0], zz]))
    m["wlora_a"] = f(np.vstack([zz, inputs["rwkv_w_a"][0]]))
    m["wg"] = f(inputs["rwkv_w_g"][0])
    m["lng_b"] = f(np.broadcast_to(inputs["rwkv_ln_g"][0], (128, 1024)))
    m["lnb_b"] = f(np.broadcast_to(inputs["rwkv_ln_b"][0], (128, 1024)))
    m["alog_b"] = f(np.broadcast_to(inputs["ssd_a_log"][0], (128, 32)))
    m["dskip_b"] = f(np.broadcast_to(inputs["ssd_d"][0], (128, 32)))
    m["normg_b"] = f(np.broadcast_to(inputs["ssd_norm_g"][0], (128, 2048)))
    m["w_up_rwkv"] = f(inputs["w_up_rwkv"][0])
    m["w_up_ssd"] = f(inputs["w_up_ssd"][0])
    m["w_out"] = f(inputs["w_out"][0])
    m["msel"] = f(np.broadcast_to(np.array([1.0 - hh, float(hh)], np.float32), (128, 2)))
    m["ffn_gT"] = f(inputs["ffn_norm_g"][0].reshape(8, 128).T)
    m["fin_b"] = f(np.broadcast_to(inputs["final_norm_g"], (128, 1024)))
    m["w_router"] = f(np.concatenate([inputs["w_router_group"][0], inputs["w_router_expert"][0]], axis=1))
    m["rb_b"] = f(np.broadcast_to(np.concatenate([inputs["b_router_group"][0], inputs["b_router_expert"][0]]), (128, 36)))
    m["w_exp_gate"] = f(inputs["w_exp_gate"][0])
    m["w_exp_up"] = f(inputs["w_exp_up"][0])
    m["w_exp_down"] = f(inputs["w_exp_down"][0])
    return m


_NC = None


def kernel(**inputs):
    global _NC
    inputs = {k: np.asarray(v) for k, v in inputs.items()}
    if _NC is None:
        _NC = build_program()
    shared = None
    in_maps = []
    for c in range(8):
        m = host_prep(inputs, c)
        if shared is None:
            shared = {k: m[k] for k in m if k not in ("xb", "msel")}
        else:
            for k in shared:
                m[k] = shared[k]
        in_maps.append(m)
    res = run_bass_kernel_spmd(_NC, in_maps, core_ids=list(range(8)))
    if DEBUG:
        return res
    out = np.empty((4, SEQ, DM), np.float32)
    for c in range(8):
        b, hh = c // 2, c % 2
        out[b, hh * 4096:(hh + 1) * 4096] = np.asarray(res.results[c]["out"])
    return out
```

```python
import numpy as np
from contextlib import ExitStack
import concourse.bass as bass
import concourse.mybir as mybir
from concourse.bass_utils import run_bass_kernel_spmd

F32 = mybir.dt.float32
BF16 = mybir.dt.bfloat16
AF = mybir.ActivationFunctionType
ALU = mybir.AluOpType
AX = mybir.AxisListType

SAME_ENGINE_SYNC = True
EMBED_WAIT = True
DEBUG = False
STOP_AFTER = 99
P2A_NT = 16
SKIP_2A = False
P2A_CHUNK = True
P2A_STAGE = 9

SEQ = 8192
DM = 1024
NT = SEQ // 128
NFM = 66
C0 = -0.6065306597126334


class Buf:
    __slots__ = ("name", "w", "r")

    def __init__(self, name=""):
        self.name = name
        self.w = None
        self.r = {}


class Ring:
    def __init__(self, tiles):
        self.tiles = tiles
        self.bufs = [Buf() for _ in tiles]
        self.i = 0

    def next(self):
        t, b = self.tiles[self.i], self.bufs[self.i]
        self.i = (self.i + 1) % len(self.tiles)
        return t, b


class Sched:
    ENG = ("pe", "act", "dve", "pool", "sp")
    R = 8

    def __init__(self, nc, es):
        self.nc = nc
        self.es = es
        self.prog = {e: [] for e in self.ENG}
        self.sems = []
        self.sem = {e: self._newsem("c_" + e) for e in self.ENG}
        self.cnt = {e: 0 for e in self.ENG}
        self.waited = {e: {} for e in self.ENG}
        self.dq = ("sp", "act", "pool")
        self.dsem = {q: [self._newsem(f"d_{q}{i}") for i in range(self.R)] for q in self.dq}
        self.dn = {q: 0 for q in self.dq}
        self.ninstr = 0
        self._streams = None
        self._cur = None

    def begin_streams(self, n):
        self._streams = [[] for _ in range(n)]

    def stream(self, k):
        self._cur = k

    def end_streams(self, proportional=False):
        streams, self._streams, self._cur = self._streams, None, None
        order = []
        if proportional:
            pos = [0] * len(streams)
            total = sum(len(x) for x in streams)
            for _ in range(total):
                best, bi = None, None
                for i, st in enumerate(streams):
                    if pos[i] < len(st):
                        f = (pos[i] + 0.5) / len(st)
                        if best is None or f < best:
                            best, bi = f, i
                order.append(streams[bi][pos[bi]])
                pos[bi] += 1
        else:
            n = max(len(x) for x in streams)
            for i in range(n):
                for st in streams:
                    if i < len(st):
                        order.append(st[i])
        for kind, args, kw in order:
            if kind == "op":
                self.op(*args, **kw)
            else:
                self.dma(*args, **kw)

    def _newsem(self, name):
        s = self.es.enter_context(self.nc.semaphore(name))
        self.sems.append(s)
        return len(self.sems) - 1

    def _emit_wait(self, e, s, v):
        if self.waited[e].get(s, 0) >= v:
            return
        self.waited[e][s] = v
        sh = self.sems[s]
        self.prog[e].append(lambda eng, sh=sh, v=v: eng.wait_ge(sh, v))
        self.ninstr += 1

    def _waits(self, e, reads, writes, embed=False):
        waits = {}
        for b in reads:
            if b.w is not None:
                s, v = b.w
                waits[s] = max(waits.get(s, 0), v)
        for b in writes:
            if b.w is not None:
                s, v = b.w
                waits[s] = max(waits.get(s, 0), v)
            for s, v in b.r.items():
                waits[s] = max(waits.get(s, 0), v)
        own = self.sem[e]
        pend = []
        for s, v in waits.items():
            if s == own and (e == "pe" or not SAME_ENGINE_SYNC):
                continue
            if self.waited[e].get(s, 0) >= v:
                continue
            pend.append((s, v))
        emb = None
        if embed and EMBED_WAIT and pend:
            emb = pend.pop()
            self.waited[e][emb[0]] = emb[1]
        for s, v in pend:
            self._emit_wait(e, s, v)
        return emb

    def op(self, e, fn, reads=(), writes=(), signal=True):
        assert signal or e == "pe"
        if self._cur is not None and self._streams is not None:
            self._streams[self._cur].append(("op", (e, fn, list(reads), list(writes)), {"signal": signal}))
            return
        emb = self._waits(e, reads, writes, embed=True)
        own = self.sem[e]
        osh = self.sems[own]
        if emb is not None:
            esh, ev = self.sems[emb[0]], emb[1]
            fn0 = fn
            fn = lambda eng, fn0=fn0, esh=esh, ev=ev: fn0(eng)._wait_ge(esh, ev)
        if signal:
            self.cnt[e] += 1
            c = self.cnt[e]
            self.prog[e].append(lambda eng: fn(eng).then_inc(osh, 1))
        else:
            c = self.cnt[e] + 1
            self.prog[e].append(lambda eng: fn(eng))
        self.ninstr += 1
        for b in writes:
            b.w = (own, c)
            b.r = {}
        for b in reads:
            b.r[own] = max(b.r.get(own, 0), c)

    def dma(self, q, out, in_, reads=(), writes=(), **kw):
        if self._cur is not None and self._streams is not None:
            kw2 = dict(kw)
            kw2["reads"] = list(reads)
            kw2["writes"] = list(writes)
            self._streams[self._cur].append(("dma", (q, out, in_), kw2))
            return
        self._waits(q, reads, writes)
        n = self.dn[q]
        self.dn[q] += 1
        s = self.dsem[q][n % self.R]
        val = 16 * (n // self.R + 1)
        sh = self.sems[s]
        if n >= self.R:
            self._emit_wait(q, s, val - 16)
        self.prog[q].append(lambda eng: eng.dma_start(out=out, in_=in_, **kw).then_inc(sh, 16))
        self.ninstr += 1
        for b in writes:
            b.w = (s, val)
            b.r = {}
        for b in reads:
            b.r[s] = max(b.r.get(s, 0), val)

    def barrier(self):
        toks = [(self.sem[e], self.cnt[e]) for e in self.ENG if self.cnt[e] > 0]
        for q in self.dq:
            n = self.dn[q]
            for i in range(self.R):
                uses = (n - i + self.R - 1) // self.R if n > i else 0
                if uses > 0:
                    toks.append((self.dsem[q][i], 16 * uses))
        for e in self.ENG:
            for s, v in toks:
                if s == self.sem[e]:
                    continue
                self._emit_wait(e, s, v)

    def act(self, out, in_, func, reads, writes, **kw):
        self.op("act", lambda e: e.activation(out=out, in_=in_, func=func, **kw), reads, writes)

    def tt(self, eng, out, in0, in1, op, reads, writes):
        self.op(eng, lambda e: e.tensor_tensor(out=out, in0=in0, in1=in1, op=op), reads, writes)

    def ts(self, eng, out, in0, s1, s2, op0, op1, reads, writes):
        if s2 is None:
            self.op(eng, lambda e: e.tensor_scalar(out=out, in0=in0, scalar1=s1, scalar2=None, op0=op0), reads, writes)
        else:
            self.op(eng, lambda e: e.tensor_scalar(out=out, in0=in0, scalar1=s1, scalar2=s2, op0=op0, op1=op1), reads, writes)

    def stt(self, eng, out, in0, scalar, in1, op0, op1, reads, writes):
        self.op(eng, lambda e: e.scalar_tensor_tensor(out=out, in0=in0, scalar=scalar, in1=in1, op0=op0, op1=op1), reads, writes)

    def copy(self, eng, out, in_, reads, writes):
        if eng == "act":
            self.op(eng, lambda e: e.activation(out=out, in_=in_, func=AF.Copy), reads, writes)
        else:
            self.op(eng, lambda e: e.tensor_copy(out=out, in_=in_), reads, writes)

    def memset(self, eng, ap, val, writes):
        self.op(eng, lambda e: e.memset(ap, val), (), writes)

    def mm(self, out, lhsT, rhs, start, stop, reads, writes, signal=True):
        self.op("pe", lambda e: e.matmul(out, lhsT=lhsT, rhs=rhs, start=start, stop=stop), reads, writes, signal=signal)

    def tr(self, out, in_, ident, reads, writes, signal=True):
        self.op("pe", lambda e: e.transpose(out=out, in_=in_, identity=ident), reads, writes, signal=signal)

    def reduce(self, eng, out, in_, op, reads, writes):
        self.op(eng, lambda e: e.tensor_reduce(out=out, in_=in_, axis=AX.X, op=op), reads, writes)

    def scan(self, out, d0, d1, init, op0, op1, reads, writes):
        self.op("dve", lambda e: e.tensor_tensor_scan(out=out, data0=d0, data1=d1, initial=init, op0=op0, op1=op1), reads, writes)

    def asel(self, out, in_, pattern, cmp, fill, base, cm, reads, writes):
        self.op("pool", lambda e: e.affine_select(out=out, in_=in_, pattern=pattern, compare_op=cmp, fill=fill, base=base, channel_multiplier=cm), reads, writes)

    def build(self):
        nc = self.nc
        with nc.Block() as block:
            @block.tensor
            def _(e):
                for f in self.prog["pe"]:
                    f(e)

            @block.scalar
            def _(e):
                for f in self.prog["act"]:
                    f(e)

            @block.vector
            def _(e):
                for f in self.prog["dve"]:
                    f(e)

            @block.gpsimd
            def _(e):
                for f in self.prog["pool"]:
                    f(e)

            @block.sync
            def _(e):
                for f in self.prog["sp"]:
                    f(e)


class Ctx:
    pass


_UID = [0]


def _alloc(nc, es):
    _UID[0] += 1
    u = _UID[0]

    def sb(name, shape, dt):
        return es.enter_context(nc.sbuf_tensor(f"{name}_u{u}", shape, dt))

    def ps(name, shape, dt):
        return es.enter_context(nc.psum_tensor(f"{name}_u{u}", shape, dt))

    return sb, ps


def phase1(cx):
    nc, S, I, D = cx.nc, cx.S, cx.inp, cx.scr
    with ExitStack() as es:
        sb, ps = _alloc(nc, es)
        hT = sb("hT", [128, 8, SEQ], BF16)
        b_hT = [Buf() for _ in range(NT)]
        gT = sb("p1_gT", [128, 8], F32); b_g = Buf()
        pv = sb("p1_pv", [128, NFM, 5], F32); b_pv = Buf()
        dtb = sb("p1_dtb", [128, 32], F32); b_dtb = Buf()
        S.dma("sp", gT[:], I["attn_gT"], writes=[b_g])
        S.dma("sp", pv[:], I["pv_fm"], writes=[b_pv])
        S.dma("sp", dtb[:], I["dt_bias_b"], writes=[b_dtb])
        xr = Ring([sb(f"p1_x{i}", [128, DM], F32) for i in range(3)])
        xsr = Ring([sb(f"p1_xs{i}", [128, DM], F32) for i in range(2)])
        ssr = Ring([sb(f"p1_ss{i}", [128, 2], F32) for i in range(2)])
        banks = Ring([ps(f"p1_bank{i}", [128, 512], F32) for i in range(8)])
        for i in range(NT):
            x_t, bx = xr.next()
            S.dma("sp", x_t[:], I["xb"][i * 128:(i + 1) * 128, :], writes=[bx])
            xs_t, bxs = xsr.next()
            ss_t, bss = ssr.next()
            S.memset("pool", ss_t[:], 0.0, [bss])
            S.act(xs_t[:], x_t[:], AF.Square, [bx], [bxs, bss], accum_out=ss_t[:, 0:1])
            S.ts("dve", ss_t[:, 1:2], ss_t[:, 0:1], 1.0 / DM, 1e-6, ALU.mult, ALU.add, [bss], [bss])
            S.act(ss_t[:, 1:2], ss_t[:, 1:2], AF.Sqrt, [bss], [bss])
            S.op("dve", lambda e, o=ss_t[:, 1:2]: e.reciprocal(out=o, in_=o), [bss], [bss])
            S.act(xs_t[:], x_t[:], AF.Copy, [bx, bss], [bxs], scale=ss_t[:, 1:2])
            p0, bp0 = banks.next()
            p1, bp1 = banks.next()
            for k in range(8):
                pp, bp = (p0, bp0) if k < 4 else (p1, bp1)
                S.tr(pp[:, (k % 4) * 128:(k % 4 + 1) * 128], xs_t[:, k * 128:(k + 1) * 128], cx.identf[:], [bxs, cx.b_const], [bp])
            for hf, (pp, bp) in enumerate(((p0, bp0), (p1, bp1))):
                S.tt("dve", hT[:, hf * 4:hf * 4 + 4, i * 128:(i + 1) * 128],
                     pp[:].rearrange("p (k t) -> p k t", t=128),
                     gT[:, hf * 4:hf * 4 + 4].unsqueeze(2).to_broadcast([128, 4, 128]),
                     ALU.mult, [bp, b_g], [b_hT[i]])
        wr = Ring([sb(f"p1_w{i}", [128, 8, 128], BF16) for i in range(2)])
        wfm = I["wfm"].rearrange("(k p) c -> p k c", p=128)
        st_l = Ring([sb(f"p1_stl{i}", [128, 516], F32) for i in range(5)])
        tmp = Ring([sb(f"p1_tmp{i}", [128, 512], F32) for i in range(4)])
        o32 = Ring([sb(f"p1_o32{i}", [128, 512], F32) for i in range(4)])
        o16 = Ring([sb(f"p1_o16{i}", [128, 512], BF16) for i in range(4)])
        print("phase1 sbuf remaining", nc.sbuf_bytes_remaining)
        for blk in range(NFM):
            w_t, bw = wr.next()
            S.dma("pool", w_t[:], wfm[:, :, blk * 128:(blk + 1) * 128], writes=[bw])
            kind = "lerp" if blk < 26 else ("conv" if blk < 50 else "gate")
            ncar = {"lerp": 1, "conv": 3, "gate": 0}[kind]
            for tt_ in range(16):
                if tt_ % 4 == 0:
                    S.begin_streams(4)
                S.stream(tt_ % 4)
                pp, bp = banks.next()
                tok = slice(tt_ * 512, (tt_ + 1) * 512)
                for k in range(8):
                    S.mm(pp[:], w_t[:, k, :], hT[:, k, tok], k == 0, k == 7,
                         [bw] + b_hT[4 * tt_:4 * tt_ + 4], [bp], signal=(k == 7))
                if kind == "gate":
                    o, bo = o16.next()
                    S.act(o[:], pp[:], AF.Sigmoid, [bp, b_pv], [bo], bias=pv[:, blk, 4:5])
                    S.dma("sp", D["GA"][blk - 50, :, tok], o[:], [bo], [cx.b_GA[blk - 50][tt_]])
                    if tt_ % 4 == 3:
                        S.end_streams()
                    continue
                st, bst = st_l.next()
                if tt_ == 0:
                    S.memset("pool", st[:, 0:ncar], 0.0, [bst])
                S.copy("act", st[:, ncar:ncar + 512], pp[:], [bp], [bst])
                nst, bnst = st_l.tiles[st_l.i], st_l.bufs[st_l.i]
                if tt_ < 15:
                    S.copy("pool", nst[:, 0:ncar], st[:, 512:512 + ncar], [bst], [bnst])
                if kind == "lerp":
                    d, bd = tmp.next()
                    S.tt("dve", d[:], st[:, 0:512], st[:, 1:513], ALU.subtract, [bst], [bd])
                    o, bo = o32.next()
                    S.stt("dve", o[:], d[:], pv[:, blk, 0:1], st[:, 1:513], ALU.mult, ALU.add, [bd, bst, b_pv], [bo])
                    S.dma("sp", D["RW"][blk, :, tok], o[:], [bo], [cx.b_RW[blk][tt_]])
                else:
                    a, ba = tmp.next()
                    S.ts("dve", a[:], st[:, 0:512], pv[:, blk, 0:1], pv[:, blk, 4:5], ALU.mult, ALU.add, [bst, b_pv], [ba])
                    for k in range(1, 4):
                        S.stt("dve", a[:], st[:, k:k + 512], pv[:, blk, k:k + 1], a[:], ALU.mult, ALU.add, [bst, ba, b_pv], [ba])
                    o, bo = o16.next()
                    S.act(o[:], a[:], AF.Silu, [ba], [bo])
                    S.dma("sp", D["XBC"][blk - 26, :, tok], o[:], [bo], [cx.b_XBC[blk - 26][tt_]])
                if tt_ % 4 == 3:
                    S.end_streams()
        wz = sb("p1_wz", [128, 8, 1024], BF16); b_wz = Buf()
        wdt = sb("p1_wdt", [128, 8, 32], BF16); b_wdt = Buf()
        S.dma("pool", wdt[:], I["wdt"].rearrange("(k p) c -> p k c", p=128), writes=[b_wdt])
        dtr = Ring([sb(f"p1_dt{i}", [128, 32], F32) for i in range(2)])
        for i in range(NT):
            tok = slice(i * 128, (i + 1) * 128)
            pp, bp = banks.next()
            for k in range(8):
                S.mm(pp[:, 0:32], hT[:, k, tok], wdt[:, k, :], k == 0, k == 7, [b_wdt, b_hT[i]], [bp], signal=(k == 7))
            d_t, bd = dtr.next()
            S.tt("dve", d_t[:], pp[:, 0:32], dtb[:], ALU.add, [bp, b_dtb], [bd])
            S.act(d_t[:], d_t[:], AF.Exp, [bd], [bd])
            S.act(d_t[:], d_t[:], AF.Ln, [bd], [bd], bias=1.0)
            S.dma("sp", D["DT"][tok, :], d_t[:], [bd], [cx.b_DT[i]])
        for zh in range(2):
            S.dma("pool", wz[:], I["wz"][:, zh * 1024:(zh + 1) * 1024].rearrange("(k p) c -> p k c", p=128), writes=[b_wz])
            for i in range(NT):
                tok = slice(i * 128, (i + 1) * 128)
                for hf in range(2):
                    pp, bp = banks.next()
                    for k in range(8):
                        S.mm(pp[:], hT[:, k, tok], wz[:, k, hf * 512:(hf + 1) * 512], k == 0, k == 7,
                             [b_wz, b_hT[i]], [bp], signal=(k == 7))
                    o, bo = o16.next()
                    S.act(o[:], pp[:], AF.Silu, [bp], [bo])
                    cz = slice(zh * 1024 + hf * 512, zh * 1024 + (hf + 1) * 512)
                    S.dma("sp", D["SZ"][tok, cz], o[:], [bo], [cx.b_SZ[i]])
    S.barrier()


class NS:
    pass


def phase2a(cx, half):
    nc, S, I, D = cx.nc, cx.S, cx.inp, cx.scr
    with ExitStack() as es:
        sb, ps = _alloc(nc, es)
        _bk = [ps(f"p2_bank{i}", [128, 512], F32) for i in range(8)]
        banks_g = [Ring(_bk[0:2]), Ring(_bk[2:4])]
        banks_t = Ring(_bk[4:6])
        banks_p = Ring(_bk[6:8])
        banks = banks_p
        bc = Buf()
        identb = sb("a_identb", [128, 128], BF16)
        S.copy("pool", identb[:], cx.identf[:], [cx.b_const], [bc])
        ones_f = sb("a_ones", [128, 128], F32)
        S.memset("pool", ones_f[:], 1.0, [bc])
        mask2 = sb("a_mask2", [128, 2, 2, 128], F32)
        mLs = sb("a_mLs", [128, 128], F32)
        mLs4 = sb("a_mLs4", [128, 4, 128], F32)
        ident4 = sb("a_ident4", [128, 4, 128], BF16)
        S.memset("pool", mask2[:], 1.0, [bc])
        S.memset("pool", mLs[:], 1.0, [bc])
        for x in range(2):
            S.asel(mask2[:, x, 0, :], mask2[:, x, 0, :], [[1, 128]], ALU.is_gt, 0.0, 0, -1, [bc], [bc])
            S.asel(mask2[:, x, 1, :], mask2[:, x, 1, :], [[1, 128]], ALU.is_ge, 0.0, 0, -1, [bc], [bc])
        S.asel(mLs[:], mLs[:], [[-1, 128]], ALU.is_gt, 0.0, 0, 1, [bc], [bc])
        for x in range(4):
            S.copy("pool", mLs4[:, x, :], mLs[:], [bc], [bc])
            S.copy("pool", ident4[:, x, :], identb[:], [bc], [bc])
        blockones = sb("a_bones", [128, 128], BF16)
        blocksel = sb("a_bsel", [128, 2], BF16)
        S.memset("pool", blockones[:], 0.0, [bc])
        S.memset("pool", blocksel[:], 0.0, [bc])
        S.memset("pool", blockones[0:64, 0:64], 1.0, [bc])
        S.memset("pool", blockones[64:128, 64:128], 1.0, [bc])
        S.memset("pool", blocksel[0:64, 0:1], 1.0, [bc])
        S.memset("pool", blocksel[64:128, 1:2], 1.0, [bc])
        pvr = sb("a_pvr", [128, 4, 5], F32)
        wl_w = sb("a_wl_w", [128, 512], BF16)
        wl_a = sb("a_wl_a", [128, 512], BF16)
        wg = sb("a_wg", [128, 512], BF16)
        lng = sb("a_lng", [128, 512], F32)
        lnb = sb("a_lnb", [128, 512], F32)
        hc = slice(512 * half, 512 * half + 512)
        S.dma("sp", pvr[:], I["pv_rw"][:, 4 * half:4 * half + 4, :], writes=[bc])
        npvr = sb("a_npvr", [128, 4, 2], F32)
        S.ts("dve", npvr[:], pvr[:, :, 0:2], -1.0, None, ALU.mult, None, [bc], [bc])
        S.dma("pool", wl_w[:], I["wlora_w"][:, hc], writes=[bc])
        S.dma("pool", wl_a[:], I["wlora_a"][:, hc], writes=[bc])
        S.dma("pool", wg[:], I["wg"][:, hc], writes=[bc])
        S.dma("sp", lng[:], I["lng_b"][:, hc], writes=[bc])
        S.dma("sp", lnb[:], I["lnb_b"][:, hc], writes=[bc])
        Sm = sb("a_Sm", [128, 4, 64], F32)
        Sb = sb("a_Sb", [128, 4, 2, 64], BF16)
        bSm = [Buf() for _ in range(4)]
        bSb = [Buf() for _ in range(2)]
        S.memset("pool", Sm[:], 0.0, bSm)
        S.memset("pool", Sb[:], 0.0, bSb)

        ws = []
        for i in range(1):
            W = NS()
            for nm in ("r", "k", "v", "sg", "a", "kkr", "rn", "kp", "beta", "cs", "ce", "P", "Pinv"):
                setattr(W, nm, sb(f"a_w{i}_{nm}", [128, 512], F32))
                setattr(W, "b_" + nm, Buf())
            W.kkn, W.b_kkn = W.kkr, W.b_kkr
            W.t1, W.b_t1 = W.ce, W.b_ce
            W.csm, W.b_csm = W.rn, W.b_rn
            W.Eend, W.b_Eend = W.a, W.b_a
            W.Pprev, W.b_Pprev = W.k, W.b_k
            W.sq = sb(f"a_w{i}_sq", [128, 512], BF16)
            W.b_sq = Buf()
            ws.append(W)
        outs = []
        for par in range(2):
            row = []
            for hp in range(4):
                O = NS()
                O.buf = Buf()
                O.aq = sb(f"a_o{par}{hp}_aq", [128, 4, 2, 128], BF16)
                for nm in ("Kh", "Bh", "vb", "rkr"):
                    setattr(O, nm, sb(f"a_o{par}{hp}_{nm}", [128, 512], BF16))
                O.btz = [sb(f"a_o{par}{hp}_btz{z}", [128, 512], BF16) for z in range(2)]
                O.ktz = [sb(f"a_o{par}{hp}_ktz{z}", [128, 512], BF16) for z in range(2)]
                for z in range(2):
                    S.memset("pool", O.btz[z][:], 0.0, [O.buf])
                    S.memset("pool", O.ktz[z][:], 0.0, [O.buf])
                O.PC = sb(f"a_o{par}{hp}_PC", [128, 4], F32)
                row.append(O)
            outs.append(row)
        shared = []
        for par in range(2):
            H = NS()
            if par == 0:
                H.zz = sb(f"a_s{par}_zz", [128, 512], F32)
                H.zg = sb(f"a_s{par}_zg", [128, 512], F32)
                H.b_zz, H.b_zg = Buf(), Buf()
            else:
                H.zz, H.zg, H.b_zz, H.b_zg = shared[0].zz, shared[0].zg, shared[0].b_zz, shared[0].b_zg
            H.tzw = sb(f"a_s{par}_tzw", [128, 512], BF16)
            H.sgz = sb(f"a_s{par}_sgz", [128, 512], BF16)
            H.buf = Buf()
            shared.append(H)

        def pre_shared(T):
            tok = slice(T * 512, (T + 1) * 512)
            H = shared[T % 2]
            S.dma("sp", H.zz[:], D["RW"][24, :, tok], [cx.b_RW[24][T]], [H.b_zz])
            S.dma("sp", H.zg[:], D["RW"][25, :, tok], [cx.b_RW[25][T]], [H.b_zg])
            S.copy("pool", H.tzw[64:128, :], H.zz[64:128, :], [H.b_zz], [H.buf])
            zt_ = H.zz[0:64, :]
            S.act(zt_, zt_, AF.Exp, [H.b_zz], [H.b_zz], scale=-2.0)
            S.ts("dve", zt_, zt_, 1.0, None, ALU.add, None, [H.b_zz], [H.b_zz])
            S.op("dve", lambda e, o=zt_: e.reciprocal(out=o, in_=o), [H.b_zz], [H.b_zz])
            S.ts("dve", zt_, zt_, 2.0, -1.0, ALU.mult, ALU.add, [H.b_zz], [H.b_zz])
            S.copy("pool", H.tzw[0:64, :], zt_, [H.b_zz], [H.buf])
            S.act(H.zg[:], H.zg[:], AF.Exp, [H.b_zg], [H.b_zg], scale=-1.0)
            S.ts("dve", H.zg[:], H.zg[:], 1.0, None, ALU.add, None, [H.b_zg], [H.b_zg])
            S.op("dve", lambda e, o=H.zg[:]: e.reciprocal(out=o, in_=o), [H.b_zg], [H.b_zg])
            S.copy("pool", H.sgz[:], H.zg[:], [H.b_zg], [H.buf])

        def v3(t):
            return t[:].rearrange("p (c t) -> p c t", t=128)

        def pre(T, hp):
            tok = slice(T * 512, (T + 1) * 512)
            O = outs[T % 2][hp]
            W = ws[hp % len(ws)]
            H = shared[T % 2]
            ghp = 4 * half + hp
            S.dma("sp", W.r[:], D["RW"][ghp, :, tok], [cx.b_RW[ghp][T]], [W.b_r])
            S.dma("sp", W.k[:], D["RW"][8 + ghp, :, tok], [cx.b_RW[8 + ghp][T]], [W.b_k])
            S.dma("sp", W.v[:], D["RW"][16 + ghp, :, tok], [cx.b_RW[16 + ghp][T]], [W.b_v])
            cs_ = slice(hp * 128, (hp + 1) * 128)
            pw, bpw = banks.next()
            S.mm(pw[:], wl_w[:, cs_], H.tzw[:], True, True, [bc, H.buf], [bpw])
            S.act(W.sg[:], pw[:], AF.Exp, [bpw, bc], [W.b_sg], bias=npvr[:, hp, 0:1], scale=-1.0)
            S.ts("dve", W.sg[:], W.sg[:], 1.0, None, ALU.add, None, [W.b_sg], [W.b_sg])
            S.op("dve", lambda e, o=W.sg[:]: e.reciprocal(out=o, in_=o), [W.b_sg], [W.b_sg])
            pa, bpa = banks.next()
            S.mm(pa[:], wl_a[:, cs_], H.tzw[:], True, True, [bc, H.buf], [bpa])
            S.act(W.a[:], pa[:], AF.Exp, [bpa, bc], [W.b_a], bias=npvr[:, hp, 1:2], scale=-1.0)
            S.ts("dve", W.a[:], W.a[:], 1.0, None, ALU.add, None, [W.b_a], [W.b_a])
            S.op("dve", lambda e, o=W.a[:]: e.reciprocal(out=o, in_=o), [W.b_a], [W.b_a])
            S.ts("dve", W.kkr[:], W.k[:], pvr[:, hp, 2:3], None, ALU.mult, None, [W.b_k, bc], [W.b_kkr])
            S.act(W.sq[:], W.kkr[:], AF.Square, [W.b_kkr], [W.b_sq])
            pss, bpss = banks.next()
            S.mm(pss[:], blockones[:], W.sq[:], True, True, [bc, W.b_sq], [bpss])
            S.ts("dve", W.rn[:], pss[:], 1e-24, None, ALU.max, None, [bpss], [W.b_rn])
            S.act(W.rn[:], W.rn[:], AF.Ln, [W.b_rn], [W.b_rn])
            S.act(W.rn[:], W.rn[:], AF.Exp, [W.b_rn], [W.b_rn], scale=-0.5)
            S.tt("dve", W.kkn[:], W.kkr[:], W.rn[:], ALU.mult, [W.b_kkr, W.b_rn, W.b_sq], [W.b_kkn])
            S.ts("dve", W.t1[:], W.a[:], -1.0, pvr[:, hp, 3:4], ALU.add, ALU.mult, [W.b_a, bc], [W.b_t1])
            S.stt("dve", W.kp[:], W.t1[:], 1.0, W.k[:], ALU.add, ALU.mult, [W.b_t1, W.b_k], [W.b_kp])
            S.tt("pool", W.beta[:], W.kkn[:], W.a[:], ALU.mult, [W.b_kkn, W.b_a], [W.b_beta])
            for c in range(4):
                cc = slice(c * 128, (c + 1) * 128)
                S.scan(W.cs[:, cc], ones_f[:, 0:128], W.sg[:, cc], 0.0, ALU.mult, ALU.add, [bc, W.b_sg], [W.b_cs])
            S.act(W.P[:], W.cs[:], AF.Exp, [W.b_cs], [W.b_P], scale=C0)
            S.act(W.Pinv[:], W.cs[:], AF.Exp, [W.b_cs], [W.b_Pinv], scale=-C0)
            S.tt("pool", W.csm[:], W.cs[:], W.sg[:], ALU.subtract, [W.b_cs, W.b_sg], [W.b_csm])
            S.act(W.Pprev[:], W.csm[:], AF.Exp, [W.b_csm], [W.b_Pprev], scale=C0)
            cs3 = v3(W.cs)
            S.tt("dve", v3(W.ce), cs3[:, :, 127:128].to_broadcast([128, 4, 128]), cs3, ALU.subtract, [W.b_cs], [W.b_ce])
            S.act(W.Eend[:], W.ce[:], AF.Exp, [W.b_ce], [W.b_Eend], scale=C0)
            S.act(O.PC[:], cs3[:, :, 127], AF.Exp, [W.b_cs], [O.buf], scale=C0)
            S.stt("dve", O.aq[:, :, 0, :], v3(W.kkn), -1.0, v3(W.Pprev), ALU.mult, ALU.mult, [W.b_kkn, W.b_Pprev], [O.buf])
            S.tt("pool", O.aq[:, :, 1, :], v3(W.r), v3(W.P), ALU.mult, [W.b_r, W.b_P], [O.buf])
            for z in range(2):
                zr = slice(64 * z, 64 * z + 64)
                S.tt("pool", O.ktz[z][zr, :], W.kp[zr, :], W.Pinv[zr, :], ALU.mult, [W.b_kp, W.b_Pinv], [O.buf])
                S.tt("pool", O.btz[z][zr, :], W.beta[zr, :], W.Pinv[zr, :], ALU.mult, [W.b_beta, W.b_Pinv], [O.buf])
            S.tt("pool", O.Kh[:], W.kp[:], W.Eend[:], ALU.mult, [W.b_kp, W.b_Eend], [O.buf])
            S.tt("pool", O.Bh[:], W.beta[:], W.Eend[:], ALU.mult, [W.b_beta, W.b_Eend], [O.buf])
            S.copy("pool", O.vb[:], W.v[:], [W.b_v], [O.buf])
            S.stt("dve", O.rkr[:], W.r[:], pvr[:, hp, 4:5], W.kp[:], ALU.mult, ALU.mult, [W.b_r, W.b_kp, bc], [O.buf])

        tokr = Ring([sb(f"a_tok{i}", [128, 3, 128], BF16) for i in range(8)])
        Dtr = Ring([sb(f"a_Dt{i}", [128, 4, 2], F32) for i in range(2)])
        ATr_g = [Ring([sb(f"a_AT{g}{i}", [128, 4, 2, 2, 128], BF16) for i in range(2)]) for g in range(2)]
        Mr_g = [Ring([sb(f"a_M{g}{i}", [128, 4, 128], BF16) for i in range(3)]) for g in range(2)]
        Nr_g = [Ring([sb(f"a_N{g}{i}", [128, 4, 128], BF16) for i in range(3)]) for g in range(2)]
        Tr_g = [Ring([sb(f"a_T{g}{i}", [128, 4, 128], BF16) for i in range(3)]) for g in range(2)]
        Wbr_g = [Ring([sb(f"a_Wb{g}{i}", [128, 4, 64], BF16) for i in range(1)]) for g in range(2)]
        Ubr_g = [Ring([sb(f"a_Ub{g}{i}", [128, 4, 64], BF16) for i in range(1)]) for g in range(2)]
        ybr = Ring([sb(f"a_yb{i}", [128, 8, 64], F32) for i in range(2)])
        bonr = Ring([sb(f"a_bon{i}", [128, 8, 64], F32) for i in range(2)])
        ycr = Ring([sb(f"a_yc{i}", [128, 8, 64], F32) for i in range(1)])
        sqr = Ring([sb(f"a_sq{i}", [128, 8, 64], F32) for i in range(1)])
        st8 = Ring([sb(f"a_st8{i}", [128, 2, 8], F32) for i in range(2)])
        yar = Ring([sb(f"a_ya{i}", [128, 512], BF16) for i in range(2)])
        yaTr = Ring([sb(f"a_yaT{i}", [128, 4, 128], BF16) for i in range(2)])
        maskflat = mask2[:].rearrange("p a b t -> p (a b t)")

        def head(T, c):
            banks = banks_p
            tc = slice(c * 128, (c + 1) * 128)
            par = T % 2
            toks = []
            Dt, bDt = Dtr.next()
            for hp in range(4):
                O = outs[par][hp]
                pb_, bpb = banks.next()
                pbv = pb_[:].bitcast(BF16)
                for j, src in enumerate((O.Kh, O.Bh, O.vb)):
                    S.tr(pbv[:, j * 128:(j + 1) * 128], src[:, tc], identb[:], [O.buf, bc], [bpb], signal=(j == 2))
                tk, btk = tokr.next()
                S.copy("act", tk[:].rearrange("p a t -> p (a t)"), pbv[:, 0:384], [bpb], [btk])
                toks.append((tk, btk))
                pd, bpd = banks.next()
                S.mm(pd[:, 0:2], O.rkr[:, tc], blocksel[:], True, True, [O.buf, bc], [bpd])
                S.copy("dve", Dt[:, hp, :], pd[:, 0:2], [bpd], [bDt])
            return toks, Dt, bDt

        def chunk(T, c, extra, hd):
            toks, Dt, bDt = hd
            tc = slice(c * 128, (c + 1) * 128)
            par = T % 2
            ci = T * 4 + c
            gtok = slice(ci * 128, (ci + 1) * 128)
            yb, byb = ybr.next()
            bon, bbon = bonr.next()
            if P2A_STAGE < 1.2:
                return
            S.begin_streams(2 + len(extra))
            for g in range(2):
                S.stream(g)
                banks = banks_g[g]
                ATr, Mr, Nr, Tr, Wbr, Ubr = ATr_g[g], Mr_g[g], Nr_g[g], Tr_g[g], Wbr_g[g], Ubr_g[g]
                heads = []
                for hl in range(4):
                    hp = 2 * g + hl // 2
                    h2 = hl % 2
                    heads.append((hl, hp, h2, slice(64 * h2, 64 * h2 + 64), outs[par][hp]))
                AT, bAT = ATr.next()
                for hl, hp, h2, pr, O in heads:
                    pA, bpA = banks.next()
                    aqh = O.aq[:, c, :, :].rearrange("p a t -> p (a t)")
                    S.mm(pA[:, 0:256], O.btz[h2][:, tc], aqh, True, True, [O.buf], [bpA], signal=False)
                    S.mm(pA[:, 256:512], O.ktz[h2][:, tc], aqh, True, True, [O.buf], [bpA])
                    S.tt("dve", AT[:, hl].rearrange("p a b t -> p (a b t)"), pA[:], maskflat, ALU.mult, [bpA, bc], [bAT])
                if P2A_STAGE < 1.5:
                    continue
                pN, bpN = banks.next()
                for hl, hp, h2, pr, O in heads:
                    S.mm(pN[:, hl * 128:(hl + 1) * 128], O.aq[:, c, 0, :], O.btz[h2][:, tc], True, True, [O.buf], [bpN], signal=(hl == 3))
                Nk, bNk = Nr.next()
                S.tt("dve", Nk[:], pN[:].rearrange("p (h t) -> p h t", t=128), mLs4[:], ALU.mult, [bpN, bc], [bNk])
                if P2A_STAGE < 1.8:
                    continue
                Tt, bTt = Tr.next()
                S.tt("pool", Tt[:], AT[:, :, 0, 0, :], ident4[:], ALU.add, [bAT, bc], [bTt])
                Mk = [AT[:, hl, 0, 0, :] for hl in range(4)]
                bMk = bAT
                for lev in range(6 if P2A_STAGE >= 3 else 0):
                    last = lev == 5
                    if not last:
                        pM, bpM = banks.next()
                        for hl in range(4):
                            S.mm(pM[:, hl * 128:(hl + 1) * 128], Nk[:, hl, :], Mk[hl], True, True, [bNk, bMk], [bpM], signal=(hl == 3))
                    pN2, bpN2 = banks.next()
                    for hl in range(4):
                        S.mm(pN2[:, hl * 128:(hl + 1) * 128], Mk[hl], Nk[:, hl, :], True, True, [bNk, bMk], [bpN2], signal=(hl == 3))
                    if not last:
                        Mn, bMn = Mr.next()
                        S.copy("act", Mn[:].rearrange("p h t -> p (h t)"), pM[:], [bpM], [bMn])
                    Nn, bNn = Nr.next()
                    S.copy("act", Nn[:].rearrange("p h t -> p (h t)"), pN2[:], [bpN2], [bNn])
                    pT, bpT = banks.next()
                    for hl in range(4):
                        S.mm(pT[:, hl * 128:(hl + 1) * 128], Nn[:, hl, :], Tt[:, hl, :], True, True, [bNn, bTt], [bpT], signal=(hl == 3))
                    Tn, bTn = Tr.next()
                    S.tt("dve", Tn[:].rearrange("p h t -> p (h t)"), pT[:], Tt[:].rearrange("p h t -> p (h t)"), ALU.add, [bpT, bTt], [bTn])
                    Tt, bTt = Tn, bTn
                    Nk, bNk = Nn, bNn
                    if not last:
                        Mk = [Mn[:, hl, :] for hl in range(4)]
                        bMk = bMn
                if P2A_STAGE < 4:
                    continue
                pW, bpW = banks.next()
                for hl, hp, h2, pr, O in heads:
                    tk, btk = toks[hp]
                    S.mm(pW[:, hl * 64:(hl + 1) * 64], O.aq[:, c, 0, :], Sb[:, hp, h2, :], True, False, [O.buf, bSb[g]], [bpW], signal=False)
                    S.mm(pW[:, hl * 64:(hl + 1) * 64], AT[:, hl, 1, 0, :], tk[:, 2, 64 * h2:64 * h2 + 64], False, True, [bAT, btk], [bpW], signal=(hl == 3))
                Wb, bWb = Wbr.next()
                S.copy("act", Wb[:].rearrange("p h i -> p (h i)"), pW[:, 0:256], [bpW], [bWb])
                pU, bpU = banks.next()
                for hl in range(4):
                    S.mm(pU[:, hl * 64:(hl + 1) * 64], Tt[:, hl, :], Wb[:, hl, :], True, True, [bTt, bWb], [bpU], signal=(hl == 3))
                Ub, bUb = Ubr.next()
                S.copy("act", Ub[:].rearrange("p h i -> p (h i)"), pU[:, 0:256], [bpU], [bUb])
                pY, bpY = banks.next()
                for hl, hp, h2, pr, O in heads:
                    tk, btk = toks[hp]
                    S.mm(pY[:, hl * 64:(hl + 1) * 64], O.aq[:, c, 1, :], Sb[:, hp, h2, :], True, False, [O.buf, bSb[g]], [bpY], signal=False)
                    S.mm(pY[:, hl * 64:(hl + 1) * 64], AT[:, hl, 0, 1, :], Ub[:, hl, :], False, False, [bAT, bUb], [bpY], signal=False)
                    S.mm(pY[:, hl * 64:(hl + 1) * 64], AT[:, hl, 1, 1, :], tk[:, 2, 64 * h2:64 * h2 + 64], False, True, [bAT, btk], [bpY], signal=(hl == 3))
                S.copy("dve", yb[:, 4 * g:4 * g + 4, :].rearrange("p h i -> p (h i)"), pY[:, 0:256], [bpY], [byb])
                for hq in range(2):
                    hp = 2 * g + hq
                    tk, btk = toks[hp]
                    S.tt("pool", bon[:, 2 * hp:2 * hp + 2, :], tk[:, 2, :].rearrange("p (h i) -> p h i", i=64),
                         Dt[:, hp, :].unsqueeze(2).to_broadcast([128, 2, 64]), ALU.mult, [btk, bDt], [bbon])
                    pS, bpS = banks.next()
                    for h2 in range(2):
                        hl = 2 * hq + h2
                        S.mm(pS[:, h2 * 64:(h2 + 1) * 64], tk[:, 1, :], Ub[:, hl, :], True, False, [btk, bUb], [bpS], signal=False)
                        S.mm(pS[:, h2 * 64:(h2 + 1) * 64], tk[:, 0, :], tk[:, 2, 64 * h2:64 * h2 + 64], False, True, [btk], [bpS], signal=(h2 == 1))
                    O = outs[par][hp]
                    for h2 in range(2):
                        pr = slice(64 * h2, 64 * h2 + 64)
                        S.stt("dve", Sm[pr, hp, :], Sm[pr, hp, :], O.PC[pr, c:c + 1], pS[pr, h2 * 64:(h2 + 1) * 64], ALU.mult, ALU.add, [bpS, O.buf, bSm[hp]], [bSm[hp]])
                for z in range(2):
                    zr = slice(64 * z, 64 * z + 64)
                    S.copy("act", Sb[zr, 2 * g:2 * g + 2, z, :], Sm[zr, 2 * g:2 * g + 2, :], [bSm[2 * g], bSm[2 * g + 1]], [bSb[g]])
            k_ = 2
            for fn_ in extra:
                S.stream(k_)
                fn_()
                k_ += 1
            S.end_streams(proportional=True)
            return dict(yb=yb, byb=byb, bon=bon, bbon=bbon, par=par, tc=tc, gtok=gtok, ci=ci)

        def tail(cc):
            banks = banks_t
            yb, byb, bon, bbon, par, tc, gtok, ci = (cc[k] for k in ("yb", "byb", "bon", "bbon", "par", "tc", "gtok", "ci"))
            s8, bs8 = st8.next()
            yc, byc = ycr.next()
            sq, bsq = sqr.next()
            S.reduce("dve", s8[:, 0, :], yb[:], ALU.add, [byb], [bs8])
            S.ts("dve", s8[:, 0, :], s8[:, 0, :], 1.0 / 64, None, ALU.mult, None, [bs8], [bs8])
            S.tt("dve", yc[:], yb[:], s8[:, 0, :].unsqueeze(2).to_broadcast([128, 8, 64]), ALU.subtract, [byb, bs8], [byc])
            S.tt("pool", sq[:], yc[:], yc[:], ALU.mult, [byc], [bsq])
            S.reduce("dve", s8[:, 1, :], sq[:], ALU.add, [bsq], [bs8])
            S.ts("dve", s8[:, 1, :], s8[:, 1, :], 1.0 / 64, 64e-5, ALU.mult, ALU.add, [bs8], [bs8])
            S.act(s8[:, 1, :], s8[:, 1, :], AF.Ln, [bs8], [bs8])
            S.act(s8[:, 1, :], s8[:, 1, :], AF.Exp, [bs8], [bs8], scale=-0.5)
            S.tt("dve", yc[:], yc[:], s8[:, 1, :].unsqueeze(2).to_broadcast([128, 8, 64]), ALU.mult, [byc, bs8], [byc])
            ycf = yc[:].rearrange("p h i -> p (h i)")
            S.tt("pool", ycf, ycf, lng[:], ALU.mult, [byc, bc], [byc])
            S.tt("pool", ycf, ycf, lnb[:], ALU.add, [byc, bc], [byc])
            S.tt("pool", ycf, ycf, bon[:].rearrange("p h i -> p (h i)"), ALU.add, [byc, bbon], [byc])
            H = shared[par]
            pg, bpg = banks.next()
            S.mm(pg[:], H.sgz[:, tc], wg[:], True, True, [H.buf, bc], [bpg])
            ya, bya = yar.next()
            S.tt("dve", ya[:], ycf, pg[:], ALU.mult, [byc, bpg], [bya])
            pb_, bpb = banks.next()
            pbv = pb_[:].bitcast(BF16)
            for j in range(4):
                S.tr(pbv[:, j * 128:(j + 1) * 128], ya[:, j * 128:(j + 1) * 128], identb[:], [bya, bc], [bpb], signal=(j == 3))
            yaT, byaT = yaTr.next()
            S.copy("act", yaT[:].rearrange("p j t -> p (j t)"), pbv[:, 0:512], [bpb], [byaT])
            S.dma("sp", D["YAT"][4 * half:4 * half + 4, :, gtok].rearrange("j p t -> p j t"), yaT[:], [byaT], [cx.b_YAT[ci]])

        print("phase2a sbuf remaining", nc.sbuf_bytes_remaining)
        pre_shared(0)
        for hp in range(4):
            pre(0, hp)
        prev = None
        hd = head(0, 0)
        nxt = {}
        for T in range(16):
            for c in range(4):
                extra = []
                if prev is not None:
                    extra.append(lambda p_=prev: tail(p_))
                ci = T * 4 + c
                has_next = ci + 1 < 64
                Tn, cn = divmod(ci + 1, 4)

                def prestream(T_=T, c_=c, Tn_=Tn, cn_=cn, has_next_=has_next):
                    if T_ + 1 < 16:
                        if c_ == 1:
                            pre_shared(T_ + 1)
                            pre(T_ + 1, 0)
                            pre(T_ + 1, 1)
                        elif c_ >= 2:
                            pre(T_ + 1, c_)
                    if has_next_:
                        nxt["hd"] = head(Tn_, cn_)

                extra.append(prestream)
                prev = chunk(T, c, extra, hd)
                hd = nxt.get("hd")
        tail(prev)
    S.barrier()


def phase2b(cx):
    nc, S, I, D = cx.nc, cx.S, cx.inp, cx.scr
    with ExitStack() as es:
        sb, ps = _alloc(nc, es)
        _bk = [ps(f"p3_bank{i}", [128, 512], F32) for i in range(8)]
        bkX = [(_bk[2 * g], Buf()) for g in range(4)]
        bkY = [(_bk[2 * g + 1], Buf()) for g in range(4)]
        bc = Buf()
        identb = sb("b_identb", [128, 128], BF16)
        S.copy("pool", identb[:], cx.identf[:], [cx.b_const], [bc])
        maskBD = sb("b_maskBD", [128, 128], F32)
        mLs = sb("b_mLs", [128, 128], F32)
        sameblk = sb("b_sameblk", [128, 128], F32)
        cones = [sb(f"b_cones{i}", [128, 128], F32) for i in range(2)]
        mUi8 = sb("b_mUi8", [128, 8, 128], F32)
        S.memset("pool", maskBD[:], 1.0, [bc])
        S.asel(maskBD[:], maskBD[:], [[1, 128]], ALU.is_ge, 0.0, 0, -1, [bc], [bc])
        for x in range(8):
            S.copy("pool", mUi8[:, x, :], maskBD[:], [bc], [bc])
        S.memset("pool", maskBD[0:64, 64:128], 0.0, [bc])
        S.memset("pool", mLs[:], 1.0, [bc])
        S.asel(mLs[:], mLs[:], [[-1, 128]], ALU.is_gt, 0.0, 0, 1, [bc], [bc])
        S.memset("pool", sameblk[:], 0.0, [bc])
        S.memset("pool", sameblk[0:64, 0:64], 1.0, [bc])
        S.memset("pool", sameblk[64:128, 64:128], 1.0, [bc])
        for i in range(2):
            S.memset("pool", cones[i][:], 0.0, [bc])
            S.memset("pool", cones[i][64 * i:64 * i + 64, :], 1.0, [bc])
        A_b = sb("b_A", [128, 32], F32)
        dsk = sb("b_dsk", [128, 32], F32)
        normg = sb("b_normg", [128, 2048], F32)
        S.dma("sp", A_b[:], I["alog_b"], writes=[bc])
        S.dma("sp", dsk[:], I["dskip_b"], writes=[bc])
        S.dma("sp", normg[:], I["normg_b"], writes=[bc])
        S.act(A_b[:], A_b[:], AF.Exp, [bc], [bc])
        S.ts("dve", A_b[:], A_b[:], -1.0, None, ALU.mult, None, [bc], [bc])
        Hm = sb("b_Hm", [128, 4, 512], F32)
        bHm = [Buf() for _ in range(4)]
        Hs = [[sb(f"b_Hs{g}{k}", [128, 512], BF16) for k in range(2)] for g in range(4)]
        bHs = [[Buf(), Buf()] for g in range(4)]
        S.memset("pool", Hm[:], 0.0, bHm)
        zt = [[sb(f"b_z{g}{k}", [128, 512], BF16) for k in range(2)] for g in range(4)]
        bzt = [[Buf(), Buf()] for g in range(4)]
        Cz = [[sb(f"b_Cz{g}{k}", [128, 128], BF16) for k in range(2)] for g in range(4)]
        bCz = [[Buf(), Buf()] for g in range(4)]
        for g in range(4):
            for k in range(2):
                S.memset("pool", Hs[g][k][:], 0.0, [bHs[g][k]])
                S.memset("pool", zt[g][k][:], 0.0, [bzt[g][k]])
                S.memset("pool", Cz[g][k][:], 0.0, [bCz[g][k]])

        def mk(name, shape, dt, n):
            return [Ring([sb(f"b_{name}{g}_{i}", shape, dt) for i in range(n)]) for g in range(4)]

        xsFr = mk("xsF", [128, 4, 128], BF16, 2)
        BTr = mk("BT", [128, 128], BF16, 2)
        CTr = mk("CT", [128, 128], BF16, 2)
        szr = mk("sz", [128, 512], BF16, 2)
        xsr = mk("xs", [128, 512], BF16, 1)
        Btmr = mk("Btm", [128, 128], BF16, 1)
        smr = mk("sm", [128, 48], F32, 1)
        xdtr = mk("xdt", [128, 512], BF16, 1)
        R8r = mk("R8", [128, 8, 128], F32, 1)
        Dmr = mk("Dm", [128, 8, 128], BF16, 1)
        MTr = mk("MT", [128, 8, 128], BF16, 1)
        cbr = mk("cb", [128, 128], BF16, 1)
        tr_ = mk("t", [128, 512], F32, 1)
        t0r = mk("t0", [128, 512], F32, 1)
        t2r = mk("t2", [128, 512], F32, 1)
        ssr = mk("ss", [128, 2], F32, 1)
        ybr = mk("yb", [128, 512], BF16, 1)
        ybTr = mk("ybT", [128, 4, 128], BF16, 1)
        dtr = Ring([sb(f"b_dt{i}", [128, 32], F32) for i in range(3)])
        adtr = Ring([sb(f"b_adt{i}", [128, 32], F32) for i in range(2)])
        print("phase2b sbuf remaining", nc.sbuf_bytes_remaining)

        def h3(ap):
            return ap.rearrange("p (h i) -> p h i", i=64)

        for i in range(NT):
            tok = slice(i * 128, (i + 1) * 128)
            tq = i // 4
            dt_t, bdt = dtr.next()
            S.dma("sp", dt_t[:], D["DT"][tok, :], [cx.b_DT[i]], [bdt])
            adt, badt = adtr.next()
            S.tt("dve", adt[:], dt_t[:], A_b[:], ALU.mult, [bdt, bc], [badt])
            S.begin_streams(4)
            for gg in range(4):
                S.stream(gg)
                g = gg
                pX, bpX = bkX[gg]
                pY, bpY = bkY[gg]
                xsF, bxsF = xsFr[gg].next()
                S.dma("sp", xsF[:], D["XBC"][4 * gg:4 * gg + 4, :, tok].rearrange("j p t -> p j t"), [cx.b_XBC[4 * gg + j][tq] for j in range(4)], [bxsF])
                BT, bBT = BTr[gg].next()
                S.dma("sp", BT[:], D["XBC"][16 + gg, :, tok], [cx.b_XBC[16 + gg][tq]], [bBT])
                CT, bCT = CTr[gg].next()
                S.dma("sp", CT[:], D["XBC"][20 + gg, :, tok], [cx.b_XBC[20 + gg][tq]], [bCT])
                sz, bsz = szr[gg].next()
                S.dma("sp", sz[:], D["SZ"][tok, gg * 512:(gg + 1) * 512], [cx.b_SZ[i]], [bsz])
                gs = slice(8 * gg, 8 * gg + 8)
                pbv = pX[:].bitcast(BF16)
                for j in range(4):
                    S.tr(pbv[:, j * 128:(j + 1) * 128], xsF[:, j, :], identb[:], [bxsF, bc], [bpX], signal=False)
                S.tr(pbv[:, 512:640], BT[:], identb[:], [bBT, bc], [bpX])
                xs, bxs = xsr[gg].next()
                S.copy("act", xs[:], pbv[:, 0:512], [bpX], [bxs])
                Btm, bBtm = Btmr[gg].next()
                S.copy("act", Btm[:], pbv[:, 512:640], [bpX], [bBtm])
                S.mm(pY[:, 0:8], maskBD[:], adt[:, gs], True, True, [bc, badt], [bpY], signal=False)
                S.mm(pY[:, 8:16], sameblk[:], adt[:, gs], True, True, [bc, badt], [bpY], signal=False)
                S.mm(pY[:, 16:24], cones[0][:], adt[:, gs], True, True, [bc, badt], [bpY], signal=False)
                S.mm(pY[:, 24:32], cones[1][:], adt[:, gs], True, True, [bc, badt], [bpY])
                sm, bsm = smr[gg].next()
                S.copy("dve", sm[:, 0:32], pY[:, 0:32], [bpY], [bsm])
                S.tt("dve", sm[:, 40:48], sm[:, 8:16], sm[:, 0:8], ALU.subtract, [bsm], [bsm])
                S.act(sm[:, 32:40], sm[:, 0:8], AF.Exp, [bsm], [bsm])
                S.act(sm[:, 40:48], sm[:, 40:48], AF.Exp, [bsm], [bsm])
                S.act(sm[:, 16:32], sm[:, 16:32], AF.Exp, [bsm], [bsm])
                xdt, bxdt = xdtr[gg].next()
                S.tt("dve", h3(xdt[:]), h3(xs[:]), dt_t[:, gs].unsqueeze(2).to_broadcast([128, 8, 64]), ALU.mult, [bxs, bdt], [bxdt])
                for k in range(2):
                    kr = slice(64 * k, 64 * k + 64)
                    S.tt("pool", h3(zt[gg][k][kr, :]), h3(xdt[kr, :]), sm[kr, 40:48].unsqueeze(2).to_broadcast([64, 8, 64]), ALU.mult, [bxdt, bsm], [bzt[gg][k]])
                    S.copy("pool", Cz[gg][k][:, kr], CT[:, kr], [bCT], [bCz[gg][k]])
                R8, bR8 = R8r[gg].next()
                S.tt("dve", R8[:], mUi8[:], adt[:, gs].unsqueeze(2).to_broadcast([128, 8, 128]), ALU.mult, [bc, badt], [bR8])
                Dm, bDm = Dmr[gg].next()
                for hf, (pseg, bpseg) in enumerate(((pX, bpX), (pY, bpY))):
                    S.mm(pseg[:], mLs[:], R8[:, 4 * hf:4 * hf + 4, :].rearrange("p h t -> p (h t)"), True, True, [bc, bR8], [bpseg])
                    S.act(Dm[:, 4 * hf:4 * hf + 4, :].rearrange("p h t -> p (h t)"), pseg[:], AF.Exp, [bpseg], [bDm])
                S.mm(pX[:, 0:128], BT[:], CT[:], True, True, [bBT, bCT], [bpX])
                cb, bcb = cbr[gg].next()
                S.tt("dve", cb[:], pX[:, 0:128], maskBD[:], ALU.mult, [bpX, bc], [bcb])
                MT, bMT = MTr[gg].next()
                S.tt("pool", MT[:], Dm[:], cb[:].unsqueeze(1).to_broadcast([128, 8, 128]), ALU.mult, [bDm, bcb], [bMT])
                for h in range(8):
                    S.mm(pY[:, h * 64:(h + 1) * 64], MT[:, h, :], xdt[:, h * 64:(h + 1) * 64], True, True, [bMT, bxdt], [bpY], signal=(h == 7))
                t0, bt0 = t0r[gg].next()
                S.copy("dve", t0[:], pY[:], [bpY], [bt0])
                S.mm(pX[:], Cz[gg][0][:], Hs[gg][0][:], True, False, [bCz[gg][0], bHs[gg][0]], [bpX], signal=False)
                S.mm(pY[:], Btm[:], zt[gg][0][:], True, True, [bBtm, bzt[gg][0]], [bpY])
                Hg = Hm[:, gg, :]
                S.tt("dve", h3(Hg), h3(Hg), sm[:, 16:24].unsqueeze(2).to_broadcast([128, 8, 64]), ALU.mult, [bHm[gg], bsm], [bHm[gg]])
                S.tt("dve", Hg, Hg, pY[:], ALU.add, [bHm[gg], bpY], [bHm[gg]])
                S.copy("act", Hs[gg][1][:], Hg, [bHm[gg]], [bHs[gg][1]])
                S.mm(pX[:], Cz[gg][1][:], Hs[gg][1][:], False, True, [bCz[gg][1], bHs[gg][1]], [bpX])
                S.mm(pY[:], Btm[:], zt[gg][1][:], True, True, [bBtm, bzt[gg][1]], [bpY])
                S.tt("dve", h3(Hg), h3(Hg), sm[:, 24:32].unsqueeze(2).to_broadcast([128, 8, 64]), ALU.mult, [bHm[gg], bsm], [bHm[gg]])
                S.tt("dve", Hg, Hg, pY[:], ALU.add, [bHm[gg], bpY], [bHm[gg]])
                S.copy("act", Hs[gg][0][:], Hg, [bHm[gg]], [bHs[gg][0]])
                t, bt = tr_[gg].next()
                S.tt("dve", h3(t[:]), h3(pX[:]), sm[:, 32:40].unsqueeze(2).to_broadcast([128, 8, 64]), ALU.mult, [bpX, bsm], [bt])
                S.tt("pool", t[:], t[:], t0[:], ALU.add, [bt, bt0], [bt])
                t2, bt2 = t2r[gg].next()
                S.tt("pool", h3(t2[:]), h3(xs[:]), dsk[:, gs].unsqueeze(2).to_broadcast([128, 8, 64]), ALU.mult, [bxs, bc], [bt2])
                S.tt("pool", t[:], t[:], t2[:], ALU.add, [bt, bt2], [bt])
                S.tt("pool", t[:], t[:], sz[:], ALU.mult, [bt, bsz], [bt])
                ss, bss = ssr[gg].next()
                S.memset("pool", ss[:], 0.0, [bss])
                S.act(t2[:], t[:], AF.Square, [bt], [bt2, bss], accum_out=ss[:, 0:1])
                S.ts("dve", ss[:, 1:2], ss[:, 0:1], 1.0 / 512, 1e-6, ALU.mult, ALU.add, [bss], [bss])
                S.act(ss[:, 1:2], ss[:, 1:2], AF.Ln, [bss], [bss])
                S.act(ss[:, 1:2], ss[:, 1:2], AF.Exp, [bss], [bss], scale=-0.5)
                yb, byb = ybr[gg].next()
                S.stt("dve", yb[:], t[:], ss[:, 1:2], normg[:, gg * 512:(gg + 1) * 512], ALU.mult, ALU.mult, [bt, bss, bc], [byb])
                pbv2 = pY[:].bitcast(BF16)
                for j in range(4):
                    S.tr(pbv2[:, j * 128:(j + 1) * 128], yb[:, j * 128:(j + 1) * 128], identb[:], [byb, bc], [bpY], signal=(j == 3))
                ybT, bybT = ybTr[gg].next()
                S.copy("act", ybT[:].rearrange("p j t -> p (j t)"), pbv2[:, 0:512], [bpY], [bybT])
                S.dma("sp", D["YBT"][4 * gg:4 * gg + 4, :, tok].rearrange("j p t -> p j t"), ybT[:], [bybT], [cx.b_YBT[i]])
            S.end_streams()
    S.barrier()


def phase2c(cx):
    nc, S, I, D = cx.nc, cx.S, cx.inp, cx.scr
    with ExitStack() as es:
        sb, ps = _alloc(nc, es)
        banks = Ring([ps(f"c_bank{i}", [128, 512], F32) for i in range(8)])
        bc = Buf()
        wupA = sb("c_wupA", [128, 8, 1024], BF16)
        wupB = sb("c_wupB", [128, 16, 1024], BF16)
        wout = sb("c_wout", [128, 8, 1024], BF16)
        S.dma("pool", wupA[:], I["w_up_rwkv"].rearrange("(k p) c -> p k c", p=128), writes=[bc])
        for q in range(2):
            S.dma("pool", wupB[:, 8 * q:8 * q + 8, :], I["w_up_ssd"][1024 * q:1024 * q + 1024, :].rearrange("(k p) c -> p k c", p=128), writes=[bc])
        S.dma("pool", wout[:], I["w_out"].rearrange("(k p) c -> p k c", p=128), writes=[bc])
        yar = Ring([sb(f"c_ya{i}", [128, 8, 512], BF16) for i in range(2)])
        ybr = Ring([sb(f"c_yb{i}", [128, 16, 512], BF16) for i in range(2)])
        gar = Ring([sb(f"c_ga{i}", [128, 16, 512], BF16) for i in range(2)])
        mgr = Ring([sb(f"c_mg{i}", [128, 8, 512], BF16) for i in range(1)])
        t1r = Ring([sb(f"c_t1{i}", [128, 512], F32) for i in range(2)])
        t2r = Ring([sb(f"c_t2{i}", [128, 512], F32) for i in range(2)])
        xr = Ring([sb(f"c_x{i}", [128, 1024], F32) for i in range(2)])
        xnr = Ring([sb(f"c_xn{i}", [128, 1024], F32) for i in range(2)])
        print("phase2c sbuf remaining", nc.sbuf_bytes_remaining)
        def loads(T):
            tok = slice(T * 512, (T + 1) * 512)
            ya, bya = yar.next()
            S.dma("sp", ya[:], D["YAT"][:, :, tok].rearrange("j p t -> p j t"), cx.b_YAT[4 * T:4 * T + 4], [bya])
            yb, byb = ybr.next()
            S.dma("sp", yb[:], D["YBT"][:, :, tok].rearrange("j p t -> p j t"), cx.b_YBT[4 * T:4 * T + 4], [byb])
            ga, bga = gar.next()
            S.dma("sp", ga[:], D["GA"][:, :, tok].rearrange("j p t -> p j t"), [cx.b_GA[j][T] for j in range(16)], [bga])
            return ya, bya, yb, byb, ga, bga

        nxt_ld = loads(0)
        for T in range(16):
            tok = slice(T * 512, (T + 1) * 512)
            ya, bya, yb, byb, ga, bga = nxt_ld
            if T + 1 < 16:
                nxt_ld = loads(T + 1)
            mg, bmg = mgr.next()
            for ob in range(8):
                oc = slice(ob * 128, (ob + 1) * 128)
                pA, bpA = banks.next()
                for k in range(8):
                    S.mm(pA[:], wupA[:, k, oc], ya[:, k, :], k == 0, k == 7, [bc, bya], [bpA], signal=(k == 7))
                pB, bpB = banks.next()
                for k in range(16):
                    S.mm(pB[:], wupB[:, k, oc], yb[:, k, :], k == 0, k == 15, [bc, byb], [bpB], signal=(k == 15))
                t1, bt1 = t1r.next()
                S.tt("dve", t1[:], pA[:], ga[:, ob, :], ALU.mult, [bpA, bga], [bt1])
                t2, bt2 = t2r.next()
                S.tt("dve", t2[:], pB[:], ga[:, 8 + ob, :], ALU.mult, [bpB, bga], [bt2])
                S.tt("pool", mg[:, ob, :], t1[:], t2[:], ALU.add, [bt1, bt2], [bmg])
            for sub in range(4):
                ti = 4 * T + sub
                rows = slice(ti * 128, (ti + 1) * 128)
                x_t, bx = xr.next()
                S.dma("sp", x_t[:], I["xb"][rows, :], writes=[bx])
                xn, bxn = xnr.next()
                for hf in range(2):
                    hs = slice(hf * 512, (hf + 1) * 512)
                    pD, bpD = banks.next()
                    for k in range(8):
                        S.mm(pD[:], mg[:, k, sub * 128:(sub + 1) * 128], wout[:, k, hs], k == 0, k == 7, [bmg, bc], [bpD], signal=(k == 7))
                    S.tt("dve", xn[:, hs], pD[:], x_t[:, hs], ALU.add, [bpD, bx], [bxn])
                S.dma("sp", D["XN"][rows, :], xn[:], [bxn], [cx.b_XN[ti]])
    S.barrier()


def phase3(cx):
    nc, S, I, D = cx.nc, cx.S, cx.inp, cx.scr
    with ExitStack() as es:
        sb, ps = _alloc(nc, es)
        _bk = [ps(f"m_bank{i}", [128, 512], F32) for i in range(8)]
        banks = Ring(_bk)
        banks_s = [Ring(_bk[0:4]), Ring(_bk[4:8])]
        bc = Buf()
        msel = sb("m_msel", [128, 2], F32)
        fgT = sb("m_fgT", [128, 8], F32)
        finb = sb("m_finb", [128, 1024], F32)
        wr = sb("m_wr", [128, 8, 36], F32)
        rbb = sb("m_rbb", [128, 36], F32)
        S.dma("sp", msel[:], I["msel"], writes=[bc])
        S.dma("sp", fgT[:], I["ffn_gT"], writes=[bc])
        S.dma("sp", finb[:], I["fin_b"], writes=[bc])
        S.dma("sp", wr[:], I["w_router"].rearrange("(k p) c -> p k c", p=128), writes=[bc])
        S.dma("sp", rbb[:], I["rb_b"], writes=[bc])
        acc = sb("m_acc", [128, 16, 1024], F32)
        b_acc = [Buf() for _ in range(16)]
        xnT = sb("m_xnT", [128, 8, 2048], BF16)
        b_xnT = [Buf() for _ in range(16)]
        coef = sb("m_coef", [128, 16, 32], F32)
        b_coef = [Buf() for _ in range(16)]
        ldr_s = [Ring([sb(f"m_ld{q}{i}", [128, 1024], F32) for i in range(1)]) for q in range(2)]
        xsr_s = [Ring([sb(f"m_xs{q}{i}", [128, 1024], F32) for i in range(1)]) for q in range(2)]
        ssr_s = [Ring([sb(f"m_ss{q}{i}", [128, 2], F32) for i in range(2)]) for q in range(2)]
        xrr_s = [Ring([sb(f"m_xr{q}{i}", [128, 8, 128], F32) for i in range(1)]) for q in range(2)]
        rtr_s = [Ring([sb(f"m_rt{q}{i}", [128, 128], F32) for i in range(2)]) for q in range(2)]
        xsr, ssr = xsr_s[0], ssr_s[0]
        wgr = Ring([sb(f"m_wg{i}", [128, 8, 512], BF16) for i in range(2)])
        wur = Ring([sb(f"m_wu{i}", [128, 8, 512], BF16) for i in range(2)])
        wdr = Ring([sb(f"m_wd{i}", [128, 4, 1024], BF16) for i in range(2)])
        hidr = Ring([sb(f"m_hid{i}", [128, 4, 512], BF16) for i in range(2)])
        sgr = Ring([sb(f"m_sg{i}", [128, 512], F32) for i in range(2)])
        outr = xsr_s[1]
        print("phase3 sbuf remaining", nc.sbuf_bytes_remaining)
        for p in range(2):
            for i in range(16):
                if i % 2 == 0:
                    S.begin_streams(2)
                q_ = i % 2
                S.stream(q_)
                banks = banks_s[q_]
                ldr, xsr, ssr, xrr, rtr = ldr_s[q_], xsr_s[q_], ssr_s[q_], xrr_s[q_], rtr_s[q_]
                lt = p * 16 + i
                A_t, bA = ldr.next()
                S.dma("sp", A_t[:], D["XN"][lt * 128:(lt + 1) * 128, :], [cx.b_XN[lt]], [bA])
                B_t, bB = xsr.next()
                S.dma("sp", B_t[:], D["XN"][4096 + lt * 128:4096 + (lt + 1) * 128, :], [cx.b_XN[32 + lt]], [bB])
                S.ts("dve", acc[:, i, :], A_t[:], msel[:, 0:1], None, ALU.mult, None, [bA, bc], [b_acc[i]])
                S.stt("dve", acc[:, i, :], B_t[:], msel[:, 1:2], acc[:, i, :], ALU.mult, ALU.add, [bB, bc, b_acc[i]], [b_acc[i]])
                xs_t, bxs = xsr.next()
                ss, bss = ssr.next()
                S.memset("pool", ss[:], 0.0, [bss])
                S.act(xs_t[:], acc[:, i, :], AF.Square, [b_acc[i]], [bxs, bss], accum_out=ss[:, 0:1])
                S.ts("dve", ss[:, 1:2], ss[:, 0:1], 1.0 / DM, 1e-6, ALU.mult, ALU.add, [bss], [bss])
                S.act(ss[:, 1:2], ss[:, 1:2], AF.Sqrt, [bss], [bss])
                S.op("dve", lambda e, o=ss[:, 1:2]: e.reciprocal(out=o, in_=o), [bss], [bss])
                S.act(xs_t[:], acc[:, i, :], AF.Copy, [b_acc[i], bss], [bxs], scale=ss[:, 1:2])
                xr_, bxr = xrr.next()
                for hf in range(2):
                    pp, bp = banks.next()
                    for k in range(4):
                        kk = hf * 4 + k
                        S.tr(pp[:, k * 128:(k + 1) * 128], xs_t[:, kk * 128:(kk + 1) * 128], cx.identf[:], [bxs, cx.b_const], [bp], signal=(k == 3))
                    gb = fgT[:, hf * 4:hf * 4 + 4].unsqueeze(2).to_broadcast([128, 4, 128])
                    pv3 = pp[:].rearrange("p (k t) -> p k t", t=128)
                    S.tt("dve", xnT[:, hf * 4:hf * 4 + 4, i * 128:(i + 1) * 128], pv3, gb, ALU.mult, [bp, bc], [b_xnT[i]])
                    S.tt("dve", xr_[:, hf * 4:hf * 4 + 4, :], pv3, gb, ALU.mult, [bp, bc], [bxr])
                pr, bpr = banks.next()
                for k in range(8):
                    S.mm(pr[:, 0:36], xr_[:, k, :], wr[:, k, :], k == 0, k == 7, [bxr, bc], [bpr], signal=(k == 7))
                rt, brt = rtr.next()
                lg = rt[:, 0:36]
                gm = rt[:, 36:40]
                oh = rt[:, 40:44]
                pen = rt[:, 44:48]
                me = rt[:, 48:80]
                mx8 = rt[:, 80:88]
                q = rt[:, 88:96]
                c1 = rt[:, 96:128]
                S.tt("dve", lg, pr[:, 0:36], rbb[:], ALU.add, [bpr, bc], [brt])
                S.reduce("dve", gm[:, 0:1], lg[:, 0:4], ALU.max, [brt], [brt])
                S.ts("dve", gm[:, 1:2], gm[:, 0:1], -1.0, None, ALU.mult, None, [brt], [brt])
                S.ts("dve", oh, lg[:, 0:4], gm[:, 0:1], None, ALU.is_equal, None, [brt], [brt])
                S.memset("pool", gm[:, 2:3], 0.0, [brt])
                S.act(pen, lg[:, 0:4], AF.Exp, [brt], [brt], bias=gm[:, 1:2], accum_out=gm[:, 2:3])
                S.op("dve", lambda e, o=gm[:, 3:4], i_=gm[:, 2:3]: e.reciprocal(out=o, in_=i_), [brt], [brt])
                S.ts("dve", pen, oh, -1.0, 1e30, ALU.add, ALU.mult, [brt], [brt])
                S.tt("dve", me.rearrange("p (g e) -> p g e", e=8), lg[:, 4:36].rearrange("p (g e) -> p g e", e=8),
                     pen.unsqueeze(2).to_broadcast([128, 4, 8]), ALU.add, [brt], [brt])
                S.op("dve", lambda e, o=mx8, i_=me: e.max(out=o, in_=i_), [brt], [brt])
                S.tt("dve", q[:, 0:1], mx8[:, 1:2], mx8[:, 0:1], ALU.subtract, [brt], [brt])
                S.act(q[:, 1:2], q[:, 0:1], AF.Exp, [brt], [brt])
                S.ts("dve", q[:, 2:3], q[:, 1:2], 1.0, None, ALU.add, None, [brt], [brt])
                S.op("dve", lambda e, o=q[:, 2:3]: e.reciprocal(out=o, in_=o), [brt], [brt])
                S.tt("dve", q[:, 3:4], q[:, 2:3], gm[:, 3:4], ALU.mult, [brt], [brt])
                S.tt("dve", q[:, 4:5], q[:, 3:4], q[:, 1:2], ALU.mult, [brt], [brt])
                S.ts("dve", c1, me, mx8[:, 0:1], q[:, 3:4], ALU.is_equal, ALU.mult, [brt], [brt])
                S.ts("dve", me, me, mx8[:, 1:2], q[:, 4:5], ALU.is_equal, ALU.mult, [brt], [brt])
                S.tt("dve", coef[:, i, :], c1, me, ALU.add, [brt], [b_coef[i]])
                if i % 2 == 1:
                    S.end_streams()
            banks = Ring(_bk)
            xsr, ssr = xsr_s[0], ssr_s[0]
            for e_ in range(32):
                wg_, bwg = wgr.next()
                S.dma("pool", wg_[:], I["w_exp_gate"][e_].rearrange("(k p) c -> p k c", p=128), writes=[bwg])
                wu_, bwu = wur.next()
                S.dma("pool", wu_[:], I["w_exp_up"][e_].rearrange("(k p) c -> p k c", p=128), writes=[bwu])
                wd_, bwd = wdr.next()
                S.dma("pool", wd_[:], I["w_exp_down"][e_].rearrange("(k p) c -> p k c", p=128), writes=[bwd])
                for tt_ in range(4):
                    tk = slice(tt_ * 512, (tt_ + 1) * 512)
                    hid, bhid = hidr.next()
                    for hb in range(4):
                        hc = slice(hb * 128, (hb + 1) * 128)
                        pG, bpG = banks.next()
                        for k in range(8):
                            S.mm(pG[:], wg_[:, k, hc], xnT[:, k, tk], k == 0, k == 7, [bwg] + b_xnT[4 * tt_:4 * tt_ + 4], [bpG], signal=(k == 7))
                        pU, bpU = banks.next()
                        for k in range(8):
                            S.mm(pU[:], wu_[:, k, hc], xnT[:, k, tk], k == 0, k == 7, [bwu] + b_xnT[4 * tt_:4 * tt_ + 4], [bpU], signal=(k == 7))
                        sg, bsg = sgr.next()
                        S.act(sg[:], pG[:], AF.Silu, [bpG], [bsg])
                        S.tt("dve", hid[:, hb, :], sg[:], pU[:], ALU.mult, [bsg, bpU], [bhid])
                    for sub in range(4):
                        i = tt_ * 4 + sub
                        for hf in range(2):
                            hs = slice(hf * 512, (hf + 1) * 512)
                            pD, bpD = banks.next()
                            for kb in range(4):
                                S.mm(pD[:], hid[:, kb, sub * 128:(sub + 1) * 128], wd_[:, kb, hs], kb == 0, kb == 3, [bhid, bwd], [bpD], signal=(kb == 3))
                            S.stt("dve", acc[:, i, hs], pD[:], coef[:, i, e_:e_ + 1], acc[:, i, hs], ALU.mult, ALU.add, [bpD, b_coef[i], b_acc[i]], [b_acc[i]])
            for i in range(16):
                lt = p * 16 + i
                xs_t, bxs = xsr.next()
                ss, bss = ssr.next()
                S.memset("pool", ss[:], 0.0, [bss])
                S.act(xs_t[:], acc[:, i, :], AF.Square, [b_acc[i]], [bxs, bss], accum_out=ss[:, 0:1])
                S.ts("dve", ss[:, 1:2], ss[:, 0:1], 1.0 / DM, 1e-6, ALU.mult, ALU.add, [bss], [bss])
                S.act(ss[:, 1:2], ss[:, 1:2], AF.Sqrt, [bss], [bss])
                S.op("dve", lambda e, o=ss[:, 1:2]: e.reciprocal(out=o, in_=o), [bss], [bss])
                o_t, bo = outr.next()
                S.stt("dve", o_t[:], acc[:, i, :], ss[:, 1:2], finb[:], ALU.mult, ALU.mult, [b_acc[i], bss, bc], [bo])
                S.dma("sp", cx.out[lt * 128:(lt + 1) * 128, :], o_t[:], [bo], [cx.b_out])


def build_program():
    nc = bass.Bass("TRN2", target_bir_lowering=False)
    cx = Ctx()
    cx.nc = nc
    cx.inp = {}
    cx.scr = {}
    cx.outs = {}

    def inp(name, shape):
        cx.inp[name] = nc.dram_tensor(name, shape, F32, kind="ExternalInput").ap()

    def scr(name, shape, dt):
        kind = "ExternalOutput" if DEBUG else "Internal"
        cx.scr[name] = nc.dram_tensor(name, shape, dt, kind=kind).ap()

    inp("xb", [SEQ, DM])
    inp("attn_gT", [128, 8])
    inp("pv_fm", [128, NFM, 5])
    inp("dt_bias_b", [128, 32])
    inp("wfm", [DM, NFM * 128])
    inp("wz", [DM, 2048])
    inp("wdt", [DM, 32])
    inp("pv_rw", [128, 8, 5])
    inp("wlora_w", [128, 1024])
    inp("wlora_a", [128, 1024])
    inp("wg", [128, 1024])
    inp("lng_b", [128, 1024])
    inp("lnb_b", [128, 1024])
    inp("alog_b", [128, 32])
    inp("dskip_b", [128, 32])
    inp("normg_b", [128, 2048])
    inp("w_up_rwkv", [1024, 1024])
    inp("w_up_ssd", [2048, 1024])
    inp("w_out", [1024, 1024])
    inp("msel", [128, 2])
    inp("ffn_gT", [128, 8])
    inp("fin_b", [128, 1024])
    inp("w_router", [1024, 36])
    inp("rb_b", [128, 36])
    inp("w_exp_gate", [32, 1024, 512])
    inp("w_exp_up", [32, 1024, 512])
    inp("w_exp_down", [32, 512, 1024])
    scr("XN", [SEQ, 1024], F32)
    cx.b_XN = [Buf() for _ in range(NT)]
    cx.out = nc.dram_tensor("out", [4096, 1024], F32, kind="ExternalOutput").ap()
    cx.b_out = Buf()
    scr("YBT", [16, 128, SEQ], BF16)
    cx.b_YBT = [Buf() for _ in range(NT)]
    scr("YAT", [8, 128, SEQ], BF16)
    cx.b_YAT = [Buf() for _ in range(NT)]
    scr("RW", [26, 128, SEQ], F32)
    scr("XBC", [24, 128, SEQ], BF16)
    scr("GA", [16, 128, SEQ], BF16)
    scr("SZ", [SEQ, 2048], BF16)
    scr("DT", [SEQ, 32], F32)
    cx.b_RW = [[Buf() for _ in range(16)] for _ in range(26)]
    cx.b_XBC = [[Buf() for _ in range(16)] for _ in range(24)]
    cx.b_GA = [[Buf() for _ in range(16)] for _ in range(16)]
    cx.b_SZ = [Buf() for _ in range(NT)]
    cx.b_DT = [Buf() for _ in range(NT)]

    with ExitStack() as es:
        S = Sched(nc, es)
        cx.S = S
        sb, ps = _alloc(nc, es)
        cx.b_const = Buf()
        cx.identf = sb("identf", [128, 128], F32)
        S.memset("pool", cx.identf[:], 1.0, [cx.b_const])
        S.asel(cx.identf[:], cx.identf[:], [[-1, 128]], ALU.is_equal, 0.0, 0, 1, [cx.b_const], [cx.b_const])
        phase1(cx)
        for half in range(2):
            if STOP_AFTER >= 2 and not SKIP_2A:
                phase2a(cx, half)
        if STOP_AFTER >= 3:
            phase2b(cx)
        if STOP_AFTER >= 4:
            phase2c(cx)
        if STOP_AFTER >= 5:
            phase3(cx)
        outs = []
        for name in ("RW", "XBC", "GA", "SZ", "DT"):
            pass
        allb = [cx.b_out] + cx.b_XN + cx.b_YAT + cx.b_YBT + [b for l in cx.b_RW for b in l] + [b for l in cx.b_XBC for b in l] + [b for l in cx.b_GA for b in l] + cx.b_SZ + cx.b_DT
        S._waits("sp", allb, allb)
        S.build()
        print("instructions:", S.ninstr, "counts:", S.cnt, S.dn)
    return nc


def host_prep(inputs, c):
    b, hh = c // 2, c % 2
    f = lambda a: np.ascontiguousarray(a, dtype=np.float32)
    w_in = inputs["w_in"][0]
    SS0, G0 = 3328, 8480
    cols = np.concatenate([np.arange(0, 3328), np.arange(SS0 + 2048, SS0 + 5120), np.arange(G0, G0 + 2048)])
    assert cols.size == NFM * 128
    m = {}
    m["xb"] = f(inputs["x"][b])
    m["attn_gT"] = f(inputs["attn_norm_g"][0].reshape(8, 128).T)
    m["wfm"] = f(w_in[:, cols])
    m["wz"] = f(w_in[:, SS0:SS0 + 2048])
    m["wdt"] = f(w_in[:, SS0 + 5120:SS0 + 5152])
    pv = np.zeros((NFM * 128, 5), np.float32)
    pv[0:3328, 0] = inputs["rwkv_mu"][0]
    pv[3328:6400, 0:4] = inputs["ssd_conv_w"][0].T
    pv[3328:6400, 4] = inputs["ssd_conv_b"][0]
    pv[6400:, 4] = inputs["b_gate"][0]
    m["pv_fm"] = f(pv.reshape(NFM, 128, 5).transpose(1, 0, 2))
    m["dt_bias_b"] = f(np.broadcast_to(inputs["ssd_dt_bias"][0], (128, 32)))
    pvr = np.stack([inputs["rwkv_w0"][0], inputs["rwkv_a0"][0], inputs["rwkv_k_k"][0],
                    inputs["rwkv_k_a"][0], inputs["rwkv_r_k"][0].reshape(-1)], axis=1)
    m["pv_rw"] = f(pvr.reshape(8, 128, 5).transpose(1, 0, 2))
    zz = np.zeros((64, 1024), np.float32)
    m["wlora_w"] = f(np.vstack([inputs["rwkv_w_decay"][0], zz]))
    m["wlora_a"] = f(np.vstack([zz, inputs["rwkv_w_a"][0]]))
    m["wg"] = f(inputs["rwkv_w_g"][0])
    m["lng_b"] = f(np.broadcast_to(inputs["rwkv_ln_g"][0], (128, 1024)))
    m["lnb_b"] = f(np.broadcast_to(inputs["rwkv_ln_b"][0], (128, 1024)))
    m["alog_b"] = f(np.broadcast_to(inputs["ssd_a_log"][0], (128, 32)))
    m["dskip_b"] = f(np.broadcast_to(inputs["ssd_d"][0], (128, 32)))
    m["normg_b"] = f(np.broadcast_to(inputs["ssd_norm_g"][0], (128, 2048)))
    m["w_up_rwkv"] = f(inputs["w_up_rwkv"][0])
    m["w_up_ssd"] = f(inputs["w_up_ssd"][0])
    m["w_out"] = f(inputs["w_out"][0])
    m["msel"] = f(np.broadcast_to(np.array([1.0 - hh, float(hh)], np.float32), (128, 2)))
    m["ffn_gT"] = f(inputs["ffn_norm_g"][0].reshape(8, 128).T)
    m["fin_b"] = f(np.broadcast_to(inputs["final_norm_g"], (128, 1024)))
    m["w_router"] = f(np.concatenate([inputs["w_router_group"][0], inputs["w_router_expert"][0]], axis=1))
    m["rb_b"] = f(np.broadcast_to(np.concatenate([inputs["b_router_group"][0], inputs["b_router_expert"][0]]), (128, 36)))
    m["w_exp_gate"] = f(inputs["w_exp_gate"][0])
    m["w_exp_up"] = f(inputs["w_exp_up"][0])
    m["w_exp_down"] = f(inputs["w_exp_down"][0])
    return m


_NC = None


def kernel(**inputs):
    global _NC
    inputs = {k: np.asarray(v) for k, v in inputs.items()}
    if _NC is None:
        _NC = build_program()
    shared = None
    in_maps = []
    for c in range(8):
        m = host_prep(inputs, c)
        if shared is None:
            shared = {k: m[k] for k in m if k not in ("xb", "msel")}
        else:
            for k in shared:
                m[k] = shared[k]
        in_maps.append(m)
    res = run_bass_kernel_spmd(_NC, in_maps, core_ids=list(range(8)))
    if DEBUG:
        return res
    out = np.empty((4, SEQ, DM), np.float32)
    for c in range(8):
        b, hh = c // 2, c % 2
        out[b, hh * 4096:(hh + 1) * 4096] = np.asarray(res.results[c]["out"])
    return out
```
